# Optimizing a Trainium2 kernel written in Bass

```python
import jax
import jax.numpy as jnp
from jax import lax
import numpy as np

D_MODEL = 2048
BATCH = 2
SEQ = 16384
DEPTH = 2

A_HEADS = 16
A_KV_HEADS = 2
A_HEAD_DIM = 64
WINDOW = 128
ATT_BLOCK = 128
ROPE_THETA = 10000.0
B_HEADS = 8
B_KEY_DIM = 128
B_VAL_DIM = 128
GLA_CHUNK = 64
C_WIDTH = D_MODEL
C_GROUP = 16
C_GROUPS = C_WIDTH // C_GROUP
C_STATE = 64
SSM_CHUNK = 128
DT_MIN = 1e-3
DT_MAX = 1e-1
N_EXPERTS = 32
TOP_K = 4
D_FF = D_MODEL // 2
SWIGLU_LIMIT = 7.0
SWIGLU_ALPHA = 1.702
MOE_BLOCK = 128
A_Q = A_HEADS * A_HEAD_DIM
A_KV = A_KV_HEADS * A_HEAD_DIM
B_QK = B_HEADS * B_KEY_DIM
B_V = B_HEADS * B_VAL_DIM
AB_IN = A_Q + 2 * A_KV + 2 * B_QK + 2 * B_V
AB_MIX = A_Q + B_V
N_AB = (DEPTH + 1) // 2
N_C = DEPTH // 2
DN_ALPHA = (2 * DEPTH) ** 0.25
DN_BETA = (8 * DEPTH) ** -0.25
LN_EPS = 1e-5
RMS_EPS = 1e-6

kernel_name = 'hybrid_swa_hgrn2_s5_moe_deepnorm'


def layer_norm(x, g, b):
    xf = x.astype(jnp.float32)
    mu = jnp.mean(xf, axis=-1, keepdims=True)
    var = jnp.mean(jnp.square(xf - mu), axis=-1, keepdims=True)
    return ((xf - mu) * lax.rsqrt(var + LN_EPS) * g.astype(jnp.float32) + b.astype(jnp.float32)).astype(x.dtype)


def rope(x, positions):
    half = x.shape[-1] // 2
    inv_freq = ROPE_THETA ** (-jnp.arange(half, dtype=jnp.float32) / half)
    ang = positions.astype(jnp.float32)[..., None] * inv_freq
    cos = jnp.cos(ang)[:, :, None, :]
    sin = jnp.sin(ang)[:, :, None, :]
    xf = x.astype(jnp.float32)
    x1, x2 = xf[..., :half], xf[..., half:]
    return jnp.concatenate([x1 * cos - x2 * sin, x2 * cos + x1 * sin], axis=-1).astype(x.dtype)


def sliding_window_attention(q, k, v, sinks):
    bsz, seq, hq, dh = q.shape
    hkv = k.shape[2]
    grp = hq // hkv
    nb = seq // ATT_BLOCK
    qb = q.reshape(bsz, nb, ATT_BLOCK, hkv, grp, dh)

    def with_prev(t):
        tb = t.reshape(bsz, nb, ATT_BLOCK, hkv, dh)
        prev = jnp.pad(tb[:, :-1], ((0, 0), (1, 0), (0, 0), (0, 0), (0, 0)))
        return jnp.concatenate([prev, tb], axis=2)

    kb, vb = with_prev(k), with_prev(v)
    s = jnp.einsum('bnqhgd,bnshd->bnhgqs', qb, kb, preferred_element_type=jnp.float32) * (dh ** -0.5)
    qi = jnp.arange(ATT_BLOCK)[:, None]
    kj = jnp.arange(2 * ATT_BLOCK)[None, :] - ATT_BLOCK
    rel = qi - kj
    band = (rel >= 0) & (rel < WINDOW)
    has_prev = (jnp.arange(nb) > 0)[:, None, None]
    mask = band[None] & (has_prev | (kj >= 0)[None])
    s = jnp.where(mask[None, :, None, None], s, -jnp.inf)
    sink = sinks.astype(jnp.float32).reshape(hkv, grp)[None, None, :, :, None, None]
    m = jnp.maximum(jnp.max(s, axis=-1, keepdims=True), sink)
    p = jnp.exp(s - m)
    p = p / (jnp.sum(p, axis=-1, keepdims=True) + jnp.exp(sink - m))
    o = jnp.einsum('bnhgqs,bnshd->bnqhgd', p.astype(v.dtype), vb)
    return o.reshape(bsz, seq, hq * dh)


def hgrn2_recurrence(q, k, v, log_f):
    bsz, seq, nh, dk = q.shape
    dv = v.shape[-1]
    L = GLA_CHUNK
    nc = seq // L

    def chunk(t):
        return t.reshape(bsz, nc, L, nh, t.shape[-1]).transpose(0, 1, 3, 2, 4)

    q, k, v, log_f = chunk(q), chunk(k), chunk(v), chunk(log_f)
    b = jnp.cumsum(log_f, axis=3)
    b_mid = b[:, :, :, L // 2:L // 2 + 1]
    b_last = b[:, :, :, -1:]
    att = jnp.einsum('bnhtd,bnhsd->bnhts', q * jnp.exp(b - b_mid), k * jnp.exp(b_mid - b))
    causal = jnp.tril(jnp.ones((L, L), dtype=bool))
    att = jnp.where(causal, att, 0.0)
    o_intra = jnp.einsum('bnhts,bnhsv->bnhtv', att, v)
    u = jnp.einsum('bnhsd,bnhsv->bnhdv', k * jnp.exp(b_last - b), v)
    decay = jnp.exp(b_last[:, :, :, 0])

    def step(state, inp):
        dec, uc = inp
        return dec[..., None] * state + uc, state

    init = jnp.zeros((bsz, nh, dk, dv), jnp.float32)
    _, s_start = lax.scan(step, init, (decay.transpose(1, 0, 2, 3), u.transpose(1, 0, 2, 3, 4)))
    s_start = s_start.transpose(1, 0, 2, 3, 4)
    o_inter = jnp.einsum('bnhtd,bnhdv->bnhtv', q * jnp.exp(b), s_start)
    o = o_intra + o_inter
    return o.transpose(0, 1, 3, 2, 4).reshape(bsz, seq, nh, dv)


def mixer_ab(h, positions, in_w, in_b, sinks, gnorm_w, out_w, out_b, lower_bound):
    bsz, seq, _ = h.shape
    f32 = jnp.float32
    proj = h @ in_w + in_b
    cuts = [A_Q, A_Q + A_KV, A_Q + 2 * A_KV, A_Q + 2 * A_KV + B_QK,
            A_Q + 2 * A_KV + 2 * B_QK, A_Q + 2 * A_KV + 2 * B_QK + B_V]
    qa, ka, va, qb, fb, ib, gb = jnp.split(proj, cuts, axis=-1)
    qa = rope(qa.reshape(bsz, seq, A_HEADS, A_HEAD_DIM), positions)
    ka = rope(ka.reshape(bsz, seq, A_KV_HEADS, A_HEAD_DIM), positions)
    va = va.reshape(bsz, seq, A_KV_HEADS, A_HEAD_DIM)
    oa = sliding_window_attention(qa, ka, va, sinks)
    lb = lower_bound.astype(f32).reshape(B_HEADS, B_KEY_DIM)
    fgate = lb + (1.0 - lb) * jax.nn.sigmoid(fb.astype(f32).reshape(bsz, seq, B_HEADS, B_KEY_DIM))
    qry = jax.nn.silu(qb.astype(f32).reshape(bsz, seq, B_HEADS, B_KEY_DIM))
    val = ib.astype(f32).reshape(bsz, seq, B_HEADS, B_VAL_DIM)
    ob = hgrn2_recurrence(qry, 1.0 - fgate, val, jnp.log(fgate))
    ob = ob * lax.rsqrt(jnp.mean(jnp.square(ob), axis=-1, keepdims=True) + RMS_EPS) * gnorm_w.astype(f32)
    ob = ob * jax.nn.silu(gb.astype(f32).reshape(bsz, seq, B_HEADS, B_VAL_DIM))
    o = jnp.concatenate([oa, ob.reshape(bsz, seq, B_V).astype(h.dtype)], axis=-1)
    return o @ out_w + out_b


def _linear_combine(e1, e2):
    a1, b1 = e1
    a2, b2 = e2
    return a1 * a2, a2 * b1 + b2


def mixer_ssm(h, in_w, A_re, A_im, log_dt, B_re, B_im, C_re, C_im, D, glu_w, glu_b, out_w):
    bsz, seq, _ = h.shape
    f32 = jnp.float32
    u = (h @ in_w).astype(f32).reshape(bsz, seq, C_GROUPS, C_GROUP)
    lam = lax.complex(A_re.astype(f32), A_im.astype(f32))
    dt = jnp.exp(log_dt.astype(f32))[:, None]
    lam_bar = jnp.exp(lam * dt)
    b_bar = ((lam_bar - 1.0) / lam)[..., None] * lax.complex(B_re.astype(f32), B_im.astype(f32))
    c_mat = lax.complex(C_re.astype(f32), C_im.astype(f32))
    nc = seq // SSM_CHUNK
    u_chunks = u.reshape(bsz, nc, SSM_CHUNK, C_GROUPS, C_GROUP).transpose(1, 0, 2, 3, 4)

    def step(state, uc):
        bu = jnp.einsum('btgc,gpc->btgp', uc.astype(jnp.complex64), b_bar)
        a = jnp.broadcast_to(lam_bar, bu.shape)
        a_cum, x_loc = lax.associative_scan(_linear_combine, (a, bu), axis=1)
        xs = a_cum * state[:, None] + x_loc
        y = jnp.real(jnp.einsum('btgp,gcp->btgc', xs, c_mat))
        return xs[:, -1], y

    init = jnp.zeros((bsz, C_GROUPS, C_STATE), jnp.complex64)
    _, ys = lax.scan(step, init, u_chunks)
    y = ys.transpose(1, 0, 2, 3, 4).reshape(bsz, seq, C_GROUPS, C_GROUP) + D.astype(f32) * u
    y = jax.nn.gelu(y.reshape(bsz, seq, C_WIDTH)).astype(h.dtype)
    y = y * jax.nn.sigmoid(y @ glu_w + glu_b)
    return y @ out_w


def clamped_swiglu(hid):
    gate, lin = jnp.split(hid, 2, axis=-1)
    gate = jnp.minimum(gate, SWIGLU_LIMIT)
    lin = jnp.clip(lin, -SWIGLU_LIMIT, SWIGLU_LIMIT)
    return gate * jax.nn.sigmoid(SWIGLU_ALPHA * gate) * (lin + 1.0)


def moe(h, router_w, router_b, w1, b1, w2, b2):
    bsz, seq, dm = h.shape
    f32 = jnp.float32
    n_tok = bsz * seq
    ht = h.reshape(n_tok, dm)
    logits = (ht @ router_w + router_b).astype(f32)
    top_val, top_idx = lax.top_k(logits, TOP_K)
    gates = jax.nn.softmax(top_val, axis=-1)
    n_assign = n_tok * TOP_K
    flat_e = top_idx.reshape(-1)
    flat_t = jnp.repeat(jnp.arange(n_tok, dtype=jnp.int32), TOP_K)
    flat_g = gates.reshape(-1)
    order = jnp.argsort(flat_e)
    se = flat_e[order]
    counts = jnp.bincount(flat_e, length=N_EXPERTS)
    padded = (counts + MOE_BLOCK - 1) // MOE_BLOCK * MOE_BLOCK
    start_sorted = jnp.cumsum(counts) - counts
    end_padded = jnp.cumsum(padded)
    start_padded = end_padded - padded
    dest = start_padded[se] + jnp.arange(n_assign, dtype=jnp.int32) - start_sorted[se]
    n_rows = -(-(n_assign + N_EXPERTS * (MOE_BLOCK - 1)) // MOE_BLOCK) * MOE_BLOCK
    n_blocks = n_rows // MOE_BLOCK
    row_tok = jnp.zeros((n_rows,), jnp.int32).at[dest].set(flat_t[order])
    row_gate = jnp.zeros((n_rows,), f32).at[dest].set(flat_g[order])
    blk_exp = jnp.minimum(
        jnp.searchsorted(end_padded, jnp.arange(n_blocks, dtype=jnp.int32) * MOE_BLOCK, side='right'),
        N_EXPERTS - 1)

    def block_step(acc, inp):
        tok, gate, e = inp
        hid = ht[tok] @ w1[e] + b1[e]
        y = clamped_swiglu(hid) @ w2[e] + b2[e]
        return acc.at[tok].add(gate[:, None] * y.astype(f32)), None

    acc0 = jnp.zeros((n_tok, dm), f32)
    out, _ = lax.scan(block_step, acc0, (row_tok.reshape(n_blocks, MOE_BLOCK),
                                         row_gate.reshape(n_blocks, MOE_BLOCK), blk_exp))
    return out.reshape(bsz, seq, dm).astype(h.dtype)


def setup_inputs(seed: int = 0) -> dict:
    key = jax.random.key(seed)
    keys = jax.random.split(key, 32)
    f32 = jnp.float32

    def nrm(i, shape, scale):
        return scale * jax.random.normal(keys[i], shape, f32)

    positions = jnp.arange(SEQ, dtype=jnp.int32)[None, :] + jax.random.randint(keys[2], (BATCH, 1), 0, 1024, jnp.int32)
    a_im = jnp.pi * jnp.broadcast_to(jnp.arange(C_STATE, dtype=f32), (N_C, C_GROUPS, C_STATE))
    return {
        'x': nrm(0, (BATCH, SEQ, D_MODEL), 1.0),
        'c': nrm(1, (BATCH, D_MODEL), 1.0),
        'positions': positions,
        'ada_w': nrm(3, (DEPTH, D_MODEL, 6 * D_MODEL), 0.5 * D_MODEL ** -0.5),
        'ada_b': nrm(4, (DEPTH, 6 * D_MODEL), 0.01),
        'ln_g': 1.0 + nrm(5, (DEPTH, 2, D_MODEL), 0.02),
        'ln_b': nrm(6, (DEPTH, 2, D_MODEL), 0.02),
        'ab_in_w': nrm(7, (N_AB, D_MODEL, AB_IN), D_MODEL ** -0.5),
        'ab_in_b': nrm(8, (N_AB, AB_IN), 0.02),
        'ab_sinks': nrm(9, (N_AB, A_HEADS), 0.5),
        'ab_gnorm_w': 1.0 + nrm(10, (N_AB, B_HEADS, B_VAL_DIM), 0.02),
        'ab_out_w': nrm(11, (N_AB, AB_MIX, D_MODEL), DN_BETA * AB_MIX ** -0.5),
        'ab_out_b': nrm(12, (N_AB, D_MODEL), 0.02),
        'hgrn_lb_logits': nrm(13, (DEPTH + 1, B_QK), 0.1),
        'c_in_w': nrm(14, (N_C, D_MODEL, C_WIDTH), D_MODEL ** -0.5),
        'c_A_re': -0.5 + nrm(15, (N_C, C_GROUPS, C_STATE), 0.01),
        'c_A_im': a_im + nrm(16, (N_C, C_GROUPS, C_STATE), 0.01),
        'c_log_dt': jax.random.uniform(keys[17], (N_C, C_GROUPS), f32, float(np.log(DT_MIN)), float(np.log(DT_MAX))),
        'c_B_re': nrm(18, (N_C, C_GROUPS, C_STATE, C_GROUP), (2 * C_GROUP) ** -0.5),
        'c_B_im': nrm(19, (N_C, C_GROUPS, C_STATE, C_GROUP), (2 * C_GROUP) ** -0.5),
        'c_C_re': nrm(20, (N_C, C_GROUPS, C_GROUP, C_STATE), C_STATE ** -0.5),
        'c_C_im': nrm(21, (N_C, C_GROUPS, C_GROUP, C_STATE), C_STATE ** -0.5),
        'c_D': nrm(22, (N_C, C_GROUPS, C_GROUP), 1.0),
        'c_glu_w': nrm(23, (N_C, C_WIDTH, C_WIDTH), C_WIDTH ** -0.5),
        'c_glu_b': nrm(24, (N_C, C_WIDTH), 0.02),
        'c_out_w': nrm(25, (N_C, C_WIDTH, D_MODEL), DN_BETA * C_WIDTH ** -0.5),
        'router_w': nrm(26, (DEPTH, D_MODEL, N_EXPERTS), D_MODEL ** -0.5),
        'router_b': nrm(27, (DEPTH, N_EXPERTS), 0.01),
        'exp_w1': nrm(28, (DEPTH, N_EXPERTS, D_MODEL, 2 * D_FF), D_MODEL ** -0.5),
        'exp_b1': nrm(29, (DEPTH, N_EXPERTS, 2 * D_FF), 0.02),
        'exp_w2': nrm(30, (DEPTH, N_EXPERTS, D_FF, D_MODEL), DN_BETA * D_FF ** -0.5),
        'exp_b2': nrm(31, (DEPTH, N_EXPERTS, D_MODEL), 0.02),
    }


def reference(x, c, positions, ada_w, ada_b, ln_g, ln_b, ab_in_w, ab_in_b, ab_sinks, ab_gnorm_w,
              ab_out_w, ab_out_b, hgrn_lb_logits, c_in_w, c_A_re, c_A_im, c_log_dt, c_B_re, c_B_im,
              c_C_re, c_C_im, c_D, c_glu_w, c_glu_b, c_out_w, router_w, router_b, exp_w1, exp_b1,
              exp_w2, exp_b2):
    cond = jax.nn.silu(c)
    lb_all = jnp.cumsum(jax.nn.softmax(hgrn_lb_logits.astype(jnp.float32), axis=0), axis=0)
    h = x
    for layer in range(DEPTH):
        mod = cond @ ada_w[layer] + ada_b[layer]
        sh_m, sc_m, g_m, sh_f, sc_f, g_f = [m[:, None, :] for m in jnp.split(mod, 6, axis=-1)]
        xin = h * (1.0 + sc_m) + sh_m
        i = layer // 2
        if layer % 2 == 0:
            y = mixer_ab(xin, positions, ab_in_w[i], ab_in_b[i], ab_sinks[i], ab_gnorm_w[i],
                         ab_out_w[i], ab_out_b[i], lb_all[layer])
        else:
            y = mixer_ssm(xin, c_in_w[i], c_A_re[i], c_A_im[i], c_log_dt[i], c_B_re[i], c_B_im[i],
                          c_C_re[i], c_C_im[i], c_D[i], c_glu_w[i], c_glu_b[i], c_out_w[i])
        h = layer_norm(DN_ALPHA * h + (1.0 + g_m) * y, ln_g[layer, 0], ln_b[layer, 0])
        xin = h * (1.0 + sc_f) + sh_f
        y = moe(xin, router_w[layer], router_b[layer], exp_w1[layer], exp_b1[layer],
                exp_w2[layer], exp_b2[layer])
        h = layer_norm(DN_ALPHA * h + (1.0 + g_f) * y, ln_g[layer, 1], ln_b[layer, 1])
    return h
```

```python
import numpy as np
from contextlib import ExitStack
import concourse.bass as bass
import concourse.mybir as mybir
from concourse.bass_utils import run_bass_kernel_spmd

F32 = mybir.dt.float32
BF16 = mybir.dt.bfloat16
I32 = mybir.dt.int32
AF = mybir.ActivationFunctionType
ALU = mybir.AluOpType
AX = mybir.AxisListType

D = 2048
NDS = 16
EPOCH_N = 14000
DN_ALPHA = 4.0 ** 0.25
LN_EPS = 1e-5
RMS_EPS = 1e-6
NEG = -30000.0
TWO_PI = 6.283185307179586
CW1 = 6.28125
CW2 = TWO_PI - 6.28125
LSUB = 32


class Buf:
    __slots__ = ("name", "last_w", "reads")

    def __init__(self, name):
        self.name = name
        self.last_w = None
        self.reads = []


class KB:
    def __init__(self, nc):
        self.nc = nc
        self.es = ExitStack()
        self.stack = [self.es]
        self.engs = {"pe": nc.tensor, "act": nc.scalar, "dve": nc.vector,
                     "pool": nc.gpsimd, "sp": nc.sync}
        self.epoch = 0
        self.sem = {n: self.es.enter_context(nc.semaphore("s_" + n)) for n in self.engs}
        self.cnt = {n: 0 for n in self.engs}
        self.seen = {n: {} for n in self.engs}
        self.dsem = [self.es.enter_context(nc.semaphore("d%d" % i)) for i in range(NDS)]
        self.dcnt = [0] * NDS
        self.dnext = 0
        self.nid = 0
        self.ninstr = 0

    def push(self):
        s = ExitStack()
        self.stack.append(s)

    def pop(self):
        self.barrier()
        self.stack.pop().close()

    def sb(self, name, shape, dt):
        self.nid += 1
        nm = "%s_%d" % (name, self.nid)
        t = self.stack[-1].enter_context(self.nc.sbuf_tensor(nm, list(shape), dt))
        return t, Buf(nm)

    def ps(self, name, shape, dt=F32):
        self.nid += 1
        nm = "%s_%d" % (name, self.nid)
        t = self.stack[-1].enter_context(self.nc.psum_tensor(nm, list(shape), dt))
        return t, Buf(nm)

    def buf(self, name=None):
        self.nid += 1
        return Buf(name or ("b%d" % self.nid))

    def _semobj(self, key):
        return self.sem[key[1]] if key[0] == "e" else self.dsem[key[1]]

    def _maybe_epoch(self):
        if max(self.cnt.values()) < EPOCH_N:
            return
        self.barrier()
        self.epoch += 1
        for n in self.engs:
            self.sem[n] = self.es.enter_context(self.nc.semaphore("s_%s_%d" % (n, self.epoch)))
            self.cnt[n] = 0
            self.seen[n] = {k: v for k, v in self.seen[n].items() if k[0] == "d"}

    def _waits(self, en, reads, writes):
        w = {}
        own = ("e", en, self.epoch)

        def add(st, raw):
            if st is None:
                return
            k, v = st
            if k[0] == "e" and k[2] != self.epoch:
                return
            if k == own and (not raw or en in ("pe", "sp")):
                return
            if w.get(k, 0) < v:
                w[k] = v
        for b in reads:
            add(b.last_w, True)
        for b in writes:
            add(b.last_w, True)
            for r in b.reads:
                add(r, False)
        eng = self.engs[en]
        seen = self.seen[en]
        for k, v in w.items():
            if seen.get(k, 0) >= v:
                continue
            eng.wait_ge(self._semobj(k), v)
            seen[k] = v

    def _stamp(self, st, reads, writes):
        for b in reads:
            b.reads.append(st)
            if len(b.reads) > 48:
                d = {}
                for k, v in b.reads:
                    if d.get(k, 0) < v:
                        d[k] = v
                b.reads = list(d.items())
        for b in writes:
            b.last_w = st
            b.reads = []

    def op(self, en, fn, reads=(), writes=()):
        self._maybe_epoch()
        self._waits(en, reads, writes)
        ins = fn(self.engs[en])
        self.cnt[en] += 1
        ins.then_inc(self.sem[en], 1)
        self._stamp((("e", en, self.epoch), self.cnt[en]), reads, writes)
        self.ninstr += 1
        return ins

    def dma(self, en, out, in_, reads=(), writes=(), **kw):
        i = self.dnext
        self.dnext = (self.dnext + 1) % NDS
        eng = self.engs[en]
        seen = self.seen[en]
        k = ("d", i)
        if self.dcnt[i] > 0 and seen.get(k, 0) < 16 * self.dcnt[i]:
            eng.wait_ge(self.dsem[i], 16 * self.dcnt[i])
            seen[k] = 16 * self.dcnt[i]
        self._waits(en, reads, writes)
        ins = eng.dma_start(out=out, in_=in_, **kw)
        self.dcnt[i] += 1
        ins.then_inc(self.dsem[i], 16)
        self._stamp((k, 16 * self.dcnt[i]), reads, writes)
        self.ninstr += 1
        return ins

    def barrier(self):
        for en, eng in self.engs.items():
            seen = self.seen[en]
            for o in self.engs:
                ko = ("e", o, self.epoch)
                if o != en and self.cnt[o] > 0 and seen.get(ko, 0) < self.cnt[o]:
                    eng.wait_ge(self.sem[o], self.cnt[o])
                    seen[ko] = self.cnt[o]
            for i in range(NDS):
                if self.dcnt[i] > 0 and seen.get(("d", i), 0) < 16 * self.dcnt[i]:
                    eng.wait_ge(self.dsem[i], 16 * self.dcnt[i])
                    seen[("d", i)] = 16 * self.dcnt[i]

    def close(self):
        self.barrier()
        while self.stack:
            self.stack.pop().close()


def host_consts():
    c = {}
    c["identF"] = np.eye(128, dtype=np.float32)
    q = np.arange(128)[:, None]
    j = np.arange(128)[None, :]
    c["mcur"] = np.where(j <= q, 0.0, NEG).astype(np.float32)
    c["mprev"] = np.where(j > q, 0.0, NEG).astype(np.float32)
    s = np.arange(128)[:, None]
    t = np.arange(128)[None, :]
    tri = ((s // 64 == t // 64) & (s <= t)).astype(np.float32)
    mid = (t // 64) * 64 + 32
    last = (t // 64) * 64 + 63
    tri_mid = ((s // 64 == t // 64) & (s <= mid)).astype(np.float32)
    tri_last = ((s // 64 == t // 64) & (s <= last)).astype(np.float32)
    c["tri"] = tri
    c["a1"] = tri - tri_mid
    c["a2"] = tri_last - tri
    sel = np.zeros((128, 4), np.float32)
    sel[:, 0] = (s[:, 0] <= 32)
    sel[:, 1] = (s[:, 0] >= 64) & (s[:, 0] <= 96)
    sel[:, 2] = (s[:, 0] < 64)
    sel[:, 3] = (s[:, 0] >= 64)
    c["sel"] = sel
    c["invf"] = (10000.0 ** (-np.arange(32, dtype=np.float32) / 32)).astype(np.float32)
    hm = np.zeros((128, 6), np.float32)
    hm[:64, 0] = 1.0
    hm[64:, 1] = 1.0
    r = np.arange(128)
    hm[:, 2] = ((r // 16) % 2 == 0)
    hm[:, 3] = ((r // 16) % 2 == 1)
    hm[:, 4] = ((r // 32) % 2 == 0)
    hm[:, 5] = ((r // 32) % 2 == 1)
    c["hmask"] = hm
    rs = np.ones((1, 64 * LSUB), np.float32)
    rs[0, ::LSUB] = 0.0
    c["rmask"] = rs
    return c


CONST_SHAPES = {"identF": [128, 128], "mcur": [128, 128], "mprev": [128, 128], "tri": [128, 128],
                "a1": [128, 128], "a2": [128, 128], "sel": [128, 4], "invf": [32], "hmask": [128, 6],
                "rmask": [1, 64 * LSUB]}


class Ctx:
    pass


def mk_ctx(name_inputs, name_outputs):
    nc = bass.Bass("TRN2", target_bir_lowering=False)
    kb = KB(nc)
    g = Ctx()
    g.nc, g.kb = nc, kb
    g.din = {}
    for n, (shp, dt) in name_inputs.items():
        g.din[n] = nc.dram_tensor(n, list(shp), dt, kind="ExternalInput").ap()
    g.dout = {}
    for n, (shp, dt) in name_outputs.items():
        g.dout[n] = nc.dram_tensor(n, list(shp), dt, kind="ExternalOutput").ap()
    g.V = lambda fn, r=(), w=(): kb.op("dve", fn, r, w)
    g.A = lambda fn, r=(), w=(): kb.op("act", fn, r, w)
    g.P = lambda fn, r=(), w=(): kb.op("pe", fn, r, w)
    g.G = lambda fn, r=(), w=(): kb.op("pool", fn, r, w)
    g.scr = {}
    return g


def scratch(g, name, shape, dt):
    t = g.nc.dram_tensor(name, list(shape), dt, kind=("ExternalOutput" if getattr(g, "debug", False) else "Internal")).ap()
    g.scr[name] = (t, g.kb.buf(name))
    return g.scr[name]


def load_consts(g):
    kb, V = g.kb, g.V
    c = Ctx()
    g.c = c
    for n in ("identF", "mcur", "mprev", "tri", "a1", "a2", "sel", "hmask"):
        t, b = kb.sb(n, CONST_SHAPES[n], F32)
        kb.dma("sp", t[:], g.din[n], writes=[b])
        setattr(c, n, t)
        setattr(c, n + "_b", b)
    c.identH, c.identH_b = kb.sb("identH", [128, 128], BF16)
    V(lambda e: e.tensor_copy(out=c.identH[:], in_=c.identF[:]), [c.identF_b], [c.identH_b])
    c.ones1, c.ones1_b = kb.sb("ones1", [1, 128], F32)
    V(lambda e: e.memset(c.ones1[:], 1.0), [], [c.ones1_b])
    c.ones, c.ones_b = kb.sb("ones", [128, 128], F32)
    V(lambda e: e.memset(c.ones[:], 1.0), [], [c.ones_b])
    c.triH, c.triH_b = kb.sb("triH", [128, 128], BF16)
    V(lambda e: e.tensor_copy(out=c.triH[:], in_=c.tri[:]), [c.tri_b], [c.triH_b])


def colvec(g, dram_vec, name, nchunk=16):
    kb, c = g.kb, g.c
    out, ob = kb.sb(name, [128, nchunk], F32)
    kb.push()
    rows, rb = kb.sb("cv_rows", [nchunk, 128], F32)
    kb.dma("sp", rows[:], dram_vec.rearrange("(c p) -> c p", p=128), writes=[rb])
    pt, pb = kb.ps("cv_ps", [128, nchunk], F32)
    g.P(lambda e: e.transpose(out=pt[:], in_=rows[:], identity=c.identF[0:nchunk, 0:nchunk]), [rb, c.identF_b], [pb])
    g.V(lambda e: e.tensor_copy(out=out[:], in_=pt[:]), [pb], [ob])
    kb.pop()
    return out, ob


def compute_mod(g, c_row, ada_w, ada_b, mod, mod_b):
    kb, V, A, P, c = g.kb, g.V, g.A, g.P, g.c
    cT, cT_b = colvec(g, c_row, "condT")
    kb.push()
    sg, sg_b = kb.sb("sg", [128, 16], F32)
    A(lambda e: e.activation(out=sg[:], in_=cT[:], func=AF.Sigmoid), [cT_b], [sg_b])
    V(lambda e: e.tensor_tensor(out=cT[:], in0=cT[:], in1=sg[:], op=ALU.mult), [cT_b, sg_b], [cT_b])
    crep, crep_b = kb.sb("crep", [128, 16, 128], F32)
    for kc in range(16):
        V(lambda e: e.tensor_scalar(out=crep[:, kc, :], in0=c.ones[:], scalar1=cT[:, kc:kc + 1], scalar2=None, op0=ALU.mult),
          [cT_b, c.ones_b], [crep_b])
    slabs = [kb.sb("adaw", [128, 16, 512], F32) for _ in range(2)]
    brow = [kb.sb("adab", [1, 512], F32) for _ in range(2)]
    pm = [kb.ps("modps", [128, 512], F32) for _ in range(2)]
    for s in range(24):
        wt, wb = slabs[s % 2]
        bt, bb = brow[s % 2]
        pt, pb = pm[s % 2]
        kb.dma("sp", wt[:], ada_w[:, s * 512:(s + 1) * 512].rearrange("(kc p) n -> p kc n", p=128), writes=[wb])
        kb.dma("sp", bt[:], ada_b[s * 512:(s + 1) * 512].rearrange("(o n) -> o n", o=1), writes=[bb])
        for kc in range(16):
            P(lambda e: e.matmul(pt[:], lhsT=crep[:, kc, :], rhs=wt[:, kc, :], start=(kc == 0), stop=False), [crep_b, wb], [pb])
        P(lambda e: e.matmul(pt[:], lhsT=c.ones1[:], rhs=bt[:], start=False, stop=True), [c.ones1_b, bb], [pb])
        add1 = (s // 4) in (1, 2, 4, 5)
        if add1:
            V(lambda e: e.tensor_scalar(out=mod[:, s * 512:(s + 1) * 512], in0=pt[:], scalar1=1.0, scalar2=None, op0=ALU.add), [pb], [mod_b])
        else:
            A(lambda e: e.copy(out=mod[:, s * 512:(s + 1) * 512], in_=pt[:]), [pb], [mod_b])
    kb.pop()


def emit_ln(g, t, t_b, out, out_b, lng, lnb, ln_b, sq, sq_b, st, st_b):
    V = g.V
    V(lambda e: e.reduce_sum(out=st[:, 0:1], in_=t[:], axis=AX.X), [t_b], [st_b])
    V(lambda e: e.tensor_scalar(out=st[:, 1:2], in0=st[:, 0:1], scalar1=-1.0 / D, scalar2=None, op0=ALU.mult), [st_b], [st_b])
    V(lambda e: e.tensor_scalar(out=t[:], in0=t[:], scalar1=st[:, 1:2], scalar2=None, op0=ALU.add), [t_b, st_b], [t_b])
    g.G(lambda e: e.tensor_tensor(out=sq[:], in0=t[:], in1=t[:], op=ALU.mult), [t_b], [sq_b])
    V(lambda e: e.reduce_sum(out=st[:, 2:3], in_=sq[:], axis=AX.X), [sq_b], [st_b])
    V(lambda e: e.tensor_scalar(out=st[:, 3:4], in0=st[:, 2:3], scalar1=1.0 / D, scalar2=LN_EPS, op0=ALU.mult, op1=ALU.add), [st_b], [st_b])
    g.A(lambda e: e.activation(out=st[:, 3:4], in_=st[:, 3:4], func=AF.Sqrt), [st_b], [st_b])
    V(lambda e: e.reciprocal(out=st[:, 3:4], in_=st[:, 3:4]), [st_b], [st_b])
    V(lambda e: e.scalar_tensor_tensor(out=out[:], in0=t[:], scalar=st[:, 3:4], in1=lng[:], op0=ALU.mult, op1=ALU.mult), [t_b, st_b, ln_b], [out_b])
    V(lambda e: e.tensor_tensor(out=out[:], in0=out[:], in1=lnb[:], op=ALU.add), [out_b, ln_b], [out_b])


def emit_transposes(g, src, src_b, dstT, dstT_b, tps, n=16, dt_ident=None):
    c = g.c
    for r in range(0, n, 8):
        pt, pb = tps[(r // 8) % len(tps)]
        m = min(8, n - r)
        for j in range(m):
            g.P(lambda e: e.transpose(out=pt[:, j, :], in_=src[:, (r + j) * 128:(r + j + 1) * 128], identity=c.identH[:]),
                [src_b, c.identH_b], [pb])
        g.A(lambda e: e.copy(out=dstT[:, r:r + m, :], in_=pt[:, 0:m, :]), [pb], [dstT_b])


def emit_sincos(g, ang, ang_b, sin_o, cos_o, o_b, shape, tag):
    kb, V, A = g.kb, g.V, g.A
    kb.push()
    kf, kf_b = kb.sb("kf" + tag, shape, F32)
    ki, ki_b = kb.sb("ki" + tag, shape, I32)
    rr, rr_b = kb.sb("rr" + tag, shape, F32)
    V(lambda e: e.tensor_scalar(out=kf[:], in0=ang[:], scalar1=1.0 / TWO_PI, scalar2=None, op0=ALU.mult), [ang_b], [kf_b])
    V(lambda e: e.tensor_copy(out=ki[:], in_=kf[:]), [kf_b], [ki_b])
    V(lambda e: e.tensor_copy(out=kf[:], in_=ki[:]), [ki_b], [kf_b])
    V(lambda e: e.scalar_tensor_tensor(out=rr[:], in0=kf[:], scalar=-CW1, in1=ang[:], op0=ALU.mult, op1=ALU.add), [kf_b, ang_b], [rr_b])
    V(lambda e: e.scalar_tensor_tensor(out=rr[:], in0=kf[:], scalar=-CW2, in1=rr[:], op0=ALU.mult, op1=ALU.add), [kf_b, rr_b], [rr_b])
    mk_, mk_b_ = kb.sb("wm" + tag, shape, F32)

    def wrap(shift):
        V(lambda e: e.tensor_scalar(out=kf[:], in0=rr[:], scalar1=float(shift), scalar2=None, op0=ALU.add), [rr_b, o_b], [kf_b])
        V(lambda e: e.tensor_scalar(out=mk_[:], in0=kf[:], scalar1=float(np.pi), scalar2=None, op0=ALU.is_gt), [kf_b], [mk_b_])
        V(lambda e: e.scalar_tensor_tensor(out=kf[:], in0=mk_[:], scalar=-TWO_PI, in1=kf[:], op0=ALU.mult, op1=ALU.add), [mk_b_, kf_b], [kf_b])
        V(lambda e: e.tensor_scalar(out=mk_[:], in0=kf[:], scalar1=float(-np.pi), scalar2=None, op0=ALU.is_lt), [kf_b], [mk_b_])
        V(lambda e: e.scalar_tensor_tensor(out=kf[:], in0=mk_[:], scalar=TWO_PI, in1=kf[:], op0=ALU.mult, op1=ALU.add), [mk_b_, kf_b], [kf_b])
    wrap(0.0)
    A(lambda e: e.activation(out=sin_o, in_=kf[:], func=AF.Sin), [kf_b], [o_b])
    wrap(np.pi / 2)
    A(lambda e: e.activation(out=cos_o, in_=kf[:], func=AF.Sin), [kf_b], [o_b])
    kb.pop()


def linear_rows(g, ntiles, get_xT, W, bias, slabs, epilogue, wname="w"):
    kb, P, c = g.kb, g.P, g.c
    kb.push()
    wmax = max(n for _, n in slabs)
    wt, wb = kb.sb(wname, [128, 16, wmax], BF16)
    bt, bb = kb.sb(wname + "b", [1, wmax], F32)
    pm, pmb = kb.ps("lin_ps", [128, 2048], F32)
    xTs = [kb.sb("xT", [128, 16, 128], BF16) for _ in range(2)]
    for si, (c0, ncol) in enumerate(slabs):
        for q in range(0, ncol, 512):
            qn = min(512, ncol - q)
            kb.dma("pool", wt[:, :, q:q + qn], W[:, c0 + q:c0 + q + qn].rearrange("(kc p) n -> p kc n", p=128), writes=[wb])
        if bias is not None:
            kb.dma("sp", bt[:, 0:ncol], bias[c0:c0 + ncol].rearrange("(o n) -> o n", o=1), writes=[bb])
        for ti in range(ntiles):
            xT, xTb = xTs[ti % 2]
            get_xT(ti, xT, xTb)
            for q in range(0, ncol, 512):
                qn = min(512, ncol - q)
                for kc in range(16):
                    P(lambda e: e.matmul(pm[:, q:q + qn], lhsT=xT[:, kc, :], rhs=wt[:, kc, q:q + qn], start=(kc == 0),
                                         stop=(kc == 15 and bias is None)), [xTb, wb], [pmb])
                if bias is not None:
                    P(lambda e: e.matmul(pm[:, q:q + qn], lhsT=c.ones1[:], rhs=bt[:, q:q + qn], start=False, stop=True),
                      [c.ones1_b, bb], [pmb])
            epilogue(ti, si, pm, pmb, c0, ncol)
    kb.pop()


def phase_inproj(g, T, xh, W, bias, mod, mod_b, proj, proj_b):
    kb, V, A, G = g.kb, g.V, g.A, g.G
    NTH = T // 128 + 1
    kb.push()
    xs = [kb.sb("xin", [128, D], F32) for _ in range(2)]
    xb16 = [kb.sb("xb16", [128, D], BF16) for _ in range(2)]
    tps = [kb.ps("tps", [128, 8, 128], BF16) for _ in range(2)]
    ots = [kb.sb("ot", [128, 2048], F32) for _ in range(2)]
    cnt = [0]

    def get_xT(ti, xT, xTb):
        x, xb = xs[ti % 2]
        xq, xqb = xb16[ti % 2]
        kb.dma("sp", x[:], xh[ti * 128:(ti + 1) * 128, :], writes=[xb])
        V(lambda e: e.tensor_tensor(out=x[:], in0=x[:], in1=mod[:, 2048:4096], op=ALU.mult), [xb, mod_b], [xb])
        G(lambda e: e.tensor_tensor(out=xq[:], in0=x[:], in1=mod[:, 0:2048], op=ALU.add), [xb, mod_b], [xqb])
        emit_transposes(g, xq, xqb, xT, xTb, tps)

    def epi(ti, si, pm, pmb, c0, ncol):
        ot, ob = ots[cnt[0] % 2]
        cnt[0] += 1
        A(lambda e: e.copy(out=ot[:, 0:ncol], in_=pm[:, 0:ncol]), [pmb], [ob])
        kb.dma("sp", proj[ti * 128:(ti + 1) * 128, c0:c0 + ncol], ot[:, 0:ncol], reads=[ob], writes=[proj_b])

    linear_rows(g, NTH, get_xT, W, bias, [(0, 2048), (2048, 2048), (4096, 1280)], epi, "w_in")
    kb.pop()


def phase_mixer(g, T, proj, proj_b, pos2d, hv, sinks, gnorm, lbl, mix, mix_b):
    kb, V, A, P, G, c = g.kb, g.V, g.A, g.P, g.G, g.c
    NT = T // 128
    NTH = NT + 1
    kb.push()
    cosT, cs_b = kb.sb("cosT", [128, NTH, 32], F32)
    sinT, _ = kb.sb("sinT", [128, NTH, 32], F32)
    kb.push()
    pi_, pib = kb.sb("posi", [NTH, 128], I32)
    pf, pfb = kb.sb("posf", [NTH, 128], F32)
    kb.dma("sp", pi_[:], pos2d, writes=[pib])
    V(lambda e: e.tensor_copy(out=pf[:], in_=pi_[:]), [pib], [pfb])
    pps, ppsb = kb.ps("posps", [128, NTH], F32)
    P(lambda e: e.transpose(out=pps[:], in_=pf[:], identity=c.identF[0:NTH, 0:NTH]), [pfb, c.identF_b], [ppsb])
    posT, posTb = kb.sb("posT", [128, NTH], F32)
    V(lambda e: e.tensor_copy(out=posT[:], in_=pps[:]), [ppsb], [posTb])
    invf, invfb = kb.sb("invf", [128, 32], F32)
    kb.dma("sp", invf[:], g.din["invf"].partition_broadcast(128), writes=[invfb])
    ang, angb = kb.sb("ang", [128, NTH, 32], F32)
    for i in range(NTH):
        V(lambda e: e.tensor_scalar(out=ang[:, i, :], in0=invf[:], scalar1=posT[:, i:i + 1], scalar2=None, op0=ALU.mult),
          [invfb, posTb], [angb])
    emit_sincos(g, ang, angb, sinT[:], cosT[:], cs_b, [128, NTH, 32], "rope")
    kb.pop()
    hvt, hvb = kb.sb("hv", [128, 2], F32)
    kb.dma("sp", hvt[:, 0:1], hv, writes=[hvb])
    V(lambda e: e.tensor_scalar(out=hvt[:, 1:2], in0=hvt[:, 0:1], scalar1=-1.0, scalar2=-NEG, op0=ALU.add, op1=ALU.mult), [hvb], [hvb])
    mpf, mpfb = kb.sb("mprevf", [128, 128], F32)
    V(lambda e: e.tensor_scalar(out=mpf[:], in0=c.mprev[:], scalar1=hvt[:, 1:2], scalar2=None, op0=ALU.add), [c.mprev_b, hvb], [mpfb])
    masks = {}
    mk_b = kb.buf()
    for key, (m0, m1) in {"even": (c.mcur, c.mprev), "odd": (c.mprev, c.mcur), "first": (mpf, c.mcur)}.items():
        mt, _ = kb.sb("mask" + key, [128, 256], F32)
        V(lambda e: e.tensor_copy(out=mt[:, 0:128], in_=m0[:]), [c.mcur_b, c.mprev_b, mpfb], [mk_b])
        V(lambda e: e.tensor_copy(out=mt[:, 128:256], in_=m1[:]), [c.mcur_b, c.mprev_b, mpfb], [mk_b])
        masks[key] = mt
    snk, snkb = kb.sb("sinks", [128, 16], F32)
    kb.dma("sp", snk[:], sinks.partition_broadcast(128), writes=[snkb])
    gn, gnb = kb.sb("gnorm", [128, 1024], F32)
    kb.dma("sp", gn[:], gnorm.rearrange("h v -> (h v)").partition_broadcast(128), writes=[gnb])
    oml, omlb = kb.sb("oml", [128, 1024], F32)
    kb.push()
    l3, l3b = kb.sb("l3", [128, 3, 1024], F32)
    kb.dma("sp", l3[:], lbl.rearrange("a n -> (a n)").partition_broadcast(128), writes=[l3b])
    mx, mxb = kb.sb("lmx", [128, 1024], F32)
    V(lambda e: e.tensor_tensor(out=mx[:], in0=l3[:, 0, :], in1=l3[:, 1, :], op=ALU.max), [l3b], [mxb])
    V(lambda e: e.tensor_tensor(out=mx[:], in0=mx[:], in1=l3[:, 2, :], op=ALU.max), [l3b, mxb], [mxb])
    for a in range(3):
        V(lambda e: e.tensor_tensor(out=l3[:, a, :], in0=l3[:, a, :], in1=mx[:], op=ALU.subtract), [l3b, mxb], [l3b])
    A(lambda e: e.activation(out=l3[:], in_=l3[:], func=AF.Exp), [l3b], [l3b])
    V(lambda e: e.tensor_tensor(out=mx[:], in0=l3[:, 1, :], in1=l3[:, 2, :], op=ALU.add), [l3b], [mxb])
    V(lambda e: e.tensor_tensor(out=l3[:, 0, :], in0=l3[:, 0, :], in1=mx[:], op=ALU.add), [l3b, mxb], [l3b])
    V(lambda e: e.reciprocal(out=l3[:, 0, :], in_=l3[:, 0, :]), [l3b], [l3b])
    V(lambda e: e.tensor_tensor(out=oml[:], in0=mx[:], in1=l3[:, 0, :], op=ALU.mult), [l3b, mxb], [omlb])
    kb.pop()
    pts = [kb.sb("pt", [128, 5376], F32)]
    qkr, qkrb = kb.sb("qkr", [128, 18, 64], BF16)
    kT2, kT2b = kb.sb("kT2", [128, 2, 128], BF16)
    v2, v2b = kb.sb("v2", [128, 2, 128], BF16)
    r1, r1b = kb.sb("r1", [128, 18, 32], F32)
    r2, r2b = kb.sb("r2", [128, 18, 32], F32)
    qT, qTb = kb.sb("qT", [128, 8, 128], BF16)
    sm, smb = kb.sb("sm", [128, 4, 256], F32)
    pb16, pb16b = kb.sb("p16", [128, 4, 256], BF16)
    pTs, pTsb = kb.sb("pTs", [128, 8, 128], BF16)
    st4, st4b = kb.sb("st4", [128, 6, 4], F32)
    mixb, mixbb = kb.sb("mixb", [128, D], BF16)
    kk, kkb = kb.sb("kk", [128, 1024], F32)
    fg, fgb = kb.sb("fg", [128, 1024], F32)
    e1, e1b = kb.sb("e1", [128, 1024], F32)
    e2, e2b = kb.sb("e2", [128, 1024], F32)
    qs, qsb = kb.sb("qs", [128, 1024], F32)
    qe, qeb = kb.sb("qe", [128, 1024], BF16)
    ke, keb = kb.sb("ke", [128, 1024], BF16)
    kl, klb = kb.sb("kl", [128, 1024], BF16)
    vb, vbb = kb.sb("vb", [128, 1024], BF16)
    qeT, qeTb = kb.sb("qeT", [128, 8, 128], BF16)
    keT, keTb = kb.sb("keT", [128, 8, 128], BF16)
    attm, attmb = kb.sb("attm", [128, 8, 128], BF16)
    selE, selEb = kb.sb("selE", [128, 8, 4], F32)
    S, Sb = kb.sb("S", [128, 8, 128], F32)
    S1, S1b = kb.sb("S1", [128, 8, 128], F32)
    Sm0, Sm0b = kb.sb("Sm0", [128, 8, 128], BF16)
    Sm1, Sm1b = kb.sb("Sm1", [128, 8, 128], BF16)
    obs, obsb = kb.sb("obs", [128, 8, 128], F32)
    ssq, ssqb = kb.sb("ssq", [128, 8], F32)
    V(lambda e: e.memset(S[:], 0.0), [], [Sb])
    X, Xb = kb.ps("X", [128, 1024], F32)
    Y, Yb = kb.ps("Y", [128, 1024], F32)
    Z, Zb = kb.ps("Z", [128, 1024], F32)
    T1, T1b = kb.ps("T1", [128, 8, 128], BF16)
    T2, T2b = kb.ps("T2", [128, 512], F32)
    X3 = X[:].rearrange("p (h v) -> p h v", h=8)
    Y3 = Y[:].rearrange("p (h v) -> p h v", h=8)
    Z3 = Z[:].rearrange("p (h v) -> p h v", h=8)
    Z4 = Z[:].rearrange("p (h s) -> p h s", h=4)

    for i in range(NTH):
        pt, ptb = pts[0]
        slot = i % 2
        real = i >= 1
        kb.dma("sp", pt[:], proj[i * 128:(i + 1) * 128, :], writes=[ptb], reads=[proj_b])
        nh0 = 0 if real else 16
        nh = 18 - nh0
        qk = pt[:, 0:1152].rearrange("p (h d) -> p h d", h=18)
        cosb = cosT[:, i, :].unsqueeze(1).to_broadcast([128, nh, 32])
        sinb = sinT[:, i, :].unsqueeze(1).to_broadcast([128, nh, 32])
        x1 = qk[:, nh0:18, 0:32]
        x2 = qk[:, nh0:18, 32:64]
        def rope_out(c0, op):
            if real:
                V(lambda e: e.tensor_tensor(out=qkr[:, 0:16, c0:c0 + 32].rearrange("p (pr hf) d -> p hf pr d", hf=2),
                                            in0=r1[:, 0:16, :].rearrange("p (hf pr) d -> p hf pr d", hf=2),
                                            in1=r2[:, 0:16, :].rearrange("p (hf pr) d -> p hf pr d", hf=2), op=op), [r1b, r2b], [qkrb])
            V(lambda e: e.tensor_tensor(out=qkr[:, 16:18, c0:c0 + 32], in0=r1[:, 16:18, :], in1=r2[:, 16:18, :], op=op), [r1b, r2b], [qkrb])
        V(lambda e: e.tensor_tensor(out=r1[:, nh0:18, :], in0=x1, in1=cosb, op=ALU.mult), [ptb, cs_b], [r1b])
        G(lambda e: e.tensor_tensor(out=r2[:, nh0:18, :], in0=x2, in1=sinb, op=ALU.mult), [ptb, cs_b], [r2b])
        rope_out(0, ALU.subtract)
        V(lambda e: e.tensor_tensor(out=r1[:, nh0:18, :], in0=x2, in1=cosb, op=ALU.mult), [ptb, cs_b], [r1b])
        G(lambda e: e.tensor_tensor(out=r2[:, nh0:18, :], in0=x1, in1=sinb, op=ALU.mult), [ptb, cs_b], [r2b])
        rope_out(32, ALU.add)
        P(lambda e: e.transpose(out=T1[:, 0, :], in_=qkr[:, 16:18, :].rearrange("p h d -> p (h d)"), identity=c.identH[:]), [qkrb, c.identH_b], [T1b])
        A(lambda e: e.copy(out=kT2[:, slot, :], in_=T1[:, 0, :]), [T1b], [kT2b])
        A(lambda e: e.copy(out=v2[:, slot, :], in_=pt[:, 1152:1280]), [ptb], [v2b])
        A(lambda e: e.activation(out=kk[:], in_=pt[:, 2304:3328], func=AF.Sigmoid, scale=-1.0), [ptb], [kkb])
        V(lambda e: e.tensor_tensor(out=kk[:], in0=kk[:], in1=oml[:], op=ALU.mult), [kkb, omlb], [kkb])
        V(lambda e: e.tensor_scalar(out=fg[:], in0=kk[:], scalar1=-1.0, scalar2=1.0, op0=ALU.mult, op1=ALU.add), [kkb], [fgb])
        A(lambda e: e.activation(out=fg[:], in_=fg[:], func=AF.Ln), [fgb], [fgb])
        if not real:
            V(lambda e: e.tensor_scalar(out=kk[:], in0=kk[:], scalar1=hvt[:, 0:1], scalar2=None, op0=ALU.mult), [kkb, hvb], [kkb])
        for q in range(2):
            P(lambda e: e.matmul(X[:, q * 512:(q + 1) * 512], lhsT=c.a1[:], rhs=fg[:, q * 512:(q + 1) * 512], start=True, stop=True), [c.a1_b, fgb], [Xb])
            P(lambda e: e.matmul(Y[:, q * 512:(q + 1) * 512], lhsT=c.a2[:], rhs=fg[:, q * 512:(q + 1) * 512], start=True, stop=True), [c.a2_b, fgb], [Yb])
        T2s = T2[:, 0:32].rearrange("p (h f) -> p h f", h=8)
        for h in range(8):
            P(lambda e: e.matmul(T2s[:, h, :], lhsT=fg[:, h * 128:(h + 1) * 128], rhs=c.sel[:], start=True, stop=True), [fgb, c.sel_b], [T2b])
        A(lambda e: e.activation(out=selE[:], in_=T2s, func=AF.Exp), [T2b], [selEb])
        A(lambda e: e.activation(out=e1[:], in_=X[:], func=AF.Exp), [Xb], [e1b])
        A(lambda e: e.activation(out=e2[:], in_=Y[:], func=AF.Exp), [Yb], [e2b])
        V(lambda e: e.tensor_tensor(out=kl[:], in0=kk[:], in1=e2[:], op=ALU.mult), [kkb, e2b], [klb])
        A(lambda e: e.activation(out=e2[:], in_=X[:], func=AF.Exp, scale=-1.0), [Xb, klb], [e2b])
        V(lambda e: e.tensor_tensor(out=ke[:], in0=kk[:], in1=e2[:], op=ALU.mult), [kkb, e2b], [keb])
        A(lambda e: e.copy(out=vb[:], in_=pt[:, 3328:4352]), [ptb], [vbb])
        if real:
            A(lambda e: e.activation(out=qs[:], in_=pt[:, 1280:2304], func=AF.Sigmoid), [ptb], [qsb])
            G(lambda e: e.tensor_tensor(out=qs[:], in0=qs[:], in1=pt[:, 1280:2304], op=ALU.mult), [qsb, ptb], [qsb])
            V(lambda e: e.tensor_tensor(out=qe[:], in0=qs[:], in1=e1[:], op=ALU.mult), [qsb, e1b], [qeb])
            emit_transposes(g, qe, qeb, qeT, qeTb, [(T1, T1b)], n=8)
            emit_transposes(g, ke, keb, keT, keTb, [(T1, T1b)], n=8)
            for h in range(8):
                P(lambda e: e.matmul(X3[:, h, :], lhsT=keT[:, h, :], rhs=qeT[:, h, :], start=True, stop=True), [keTb, qeTb], [Xb])
            V(lambda e: e.tensor_tensor(out=attm[:], in0=X3, in1=c.tri[:].unsqueeze(1).to_broadcast([128, 8, 128]), op=ALU.mult), [Xb, c.tri_b], [attmb])
        for h in range(8):
            P(lambda e: e.matmul(Z3[:, h, :], lhsT=kl[0:64, h * 128:(h + 1) * 128], rhs=vb[0:64, h * 128:(h + 1) * 128], start=True, stop=True), [klb, vbb], [Zb])
        bc = lambda j: selE[:, :, j:j + 1].to_broadcast([128, 8, 128])
        if real:
            V(lambda e: e.tensor_tensor(out=Sm0[:], in0=S[:], in1=bc(0), op=ALU.mult), [Sb, selEb], [Sm0b])
        V(lambda e: e.tensor_tensor(out=S1[:], in0=S[:], in1=bc(2), op=ALU.mult), [Sb, selEb], [S1b])
        V(lambda e: e.tensor_tensor(out=S1[:], in0=S1[:], in1=Z3, op=ALU.add), [S1b, Zb], [S1b])
        if real:
            V(lambda e: e.tensor_tensor(out=Sm1[:], in0=S1[:], in1=bc(1), op=ALU.mult), [S1b, selEb], [Sm1b])
            for h in range(8):
                P(lambda e: e.matmul(Y3[:, h, :], lhsT=attm[:, h, :], rhs=vb[:, h * 128:(h + 1) * 128], start=True, stop=False), [attmb, vbb], [Yb])
                P(lambda e: e.matmul(Y3[0:64, h, :], lhsT=qeT[:, h, 0:64], rhs=Sm0[:, h, :], start=False, stop=True), [qeTb, Sm0b], [Yb])
                P(lambda e: e.matmul(Y3[64:128, h, :], lhsT=qeT[:, h, 64:128], rhs=Sm1[:, h, :], start=False, stop=True), [qeTb, Sm1b], [Yb])
        for h in range(8):
            P(lambda e: e.matmul(X3[:, h, :], lhsT=kl[64:128, h * 128:(h + 1) * 128], rhs=vb[64:128, h * 128:(h + 1) * 128], start=True, stop=True), [klb, vbb], [Xb])
        V(lambda e: e.tensor_tensor(out=S[:], in0=S1[:], in1=bc(3), op=ALU.mult), [S1b, selEb], [Sb])
        V(lambda e: e.tensor_tensor(out=S[:], in0=S[:], in1=X3, op=ALU.add), [Sb, Xb], [Sb])
        if not real:
            continue
        A(lambda e: e.copy(out=obs[:], in_=Y3), [Yb], [obsb])
        G(lambda e: e.tensor_tensor(out=e1[:].rearrange("p (h v) -> p h v", h=8), in0=obs[:], in1=obs[:], op=ALU.mult), [obsb], [e1b])
        V(lambda e: e.reduce_sum(out=ssq[:], in_=e1[:].rearrange("p (h v) -> p h v", h=8), axis=AX.X), [e1b], [ssqb])
        V(lambda e: e.tensor_scalar(out=ssq[:], in0=ssq[:], scalar1=1.0 / 128, scalar2=RMS_EPS, op0=ALU.mult, op1=ALU.add), [ssqb], [ssqb])
        A(lambda e: e.activation(out=ssq[:], in_=ssq[:], func=AF.Sqrt), [ssqb], [ssqb])
        V(lambda e: e.reciprocal(out=ssq[:], in_=ssq[:]), [ssqb], [ssqb])
        V(lambda e: e.tensor_tensor(out=obs[:], in0=obs[:], in1=ssq[:].unsqueeze(2).to_broadcast([128, 8, 128]), op=ALU.mult), [obsb, ssqb], [obsb])
        G(lambda e: e.tensor_tensor(out=obs[:], in0=obs[:], in1=gn[:].rearrange("p (h v) -> p h v", h=8), op=ALU.mult), [obsb, gnb], [obsb])
        A(lambda e: e.activation(out=e2[:], in_=pt[:, 4352:5376], func=AF.Sigmoid), [ptb], [e2b])
        G(lambda e: e.tensor_tensor(out=e2[:], in0=e2[:], in1=pt[:, 4352:5376], op=ALU.mult), [e2b, ptb], [e2b])
        V(lambda e: e.tensor_tensor(out=mixb[:, 1024:2048], in0=obs[:].rearrange("p h v -> p (h v)"), in1=e2[:], op=ALU.mult), [obsb, e2b], [mixbb])
        for j in range(8):
            P(lambda e: e.transpose(out=T1[:, j, :], in_=qkr[:, 2 * j:2 * j + 2, :].rearrange("p h d -> p (h d)"), identity=c.identH[:]), [qkrb, c.identH_b], [T1b])
        A(lambda e: e.copy(out=qT[:], in_=T1[:]), [T1b], [qTb])
        mt = masks["first"] if i == 1 else (masks["even"] if slot == 0 else masks["odd"])
        for g4 in range(4):
            heads = [g4 * 4 + j for j in range(4)]
            for j, h in enumerate(heads):
                pr, half = h % 8, h // 8
                for sl in range(2):
                    P(lambda e: e.matmul(Z4[:, j, sl * 128:(sl + 1) * 128], lhsT=qT[half * 64:(half + 1) * 64, pr, :],
                                         rhs=kT2[half * 64:(half + 1) * 64, sl, :], start=True, stop=True), [qTb, kT2b], [Zb])
            V(lambda e: e.scalar_tensor_tensor(out=sm[:], in0=Z4, scalar=0.125, in1=mt[:].unsqueeze(1).to_broadcast([128, 4, 256]),
                                               op0=ALU.mult, op1=ALU.add), [Zb, mk_b], [smb])
            V(lambda e: e.tensor_reduce(out=st4[:, 0, :], in_=sm[:], axis=AX.X, op=ALU.max), [smb], [st4b])
            V(lambda e: e.tensor_tensor(out=st4[:, 0, :], in0=st4[:, 0, :], in1=snk[:, g4 * 4:(g4 + 1) * 4], op=ALU.max), [st4b, snkb], [st4b])
            V(lambda e: e.tensor_tensor(out=sm[:], in0=sm[:], in1=st4[:, 0, :].unsqueeze(2).to_broadcast([128, 4, 256]), op=ALU.subtract), [smb, st4b], [smb])
            A(lambda e: e.activation(out=pb16[:], in_=sm[:], func=AF.Exp), [smb], [pb16b])
            V(lambda e: e.reduce_sum(out=st4[:, 1, :], in_=pb16[:], axis=AX.X), [pb16b], [st4b])
            V(lambda e: e.tensor_tensor(out=st4[:, 2, :], in0=snk[:, g4 * 4:(g4 + 1) * 4], in1=st4[:, 0, :], op=ALU.subtract), [st4b, snkb], [st4b])
            A(lambda e: e.activation(out=st4[:, 3, :], in_=st4[:, 2, :], func=AF.Exp), [st4b], [st4b])
            V(lambda e: e.tensor_tensor(out=st4[:, 4, :], in0=st4[:, 1, :], in1=st4[:, 3, :], op=ALU.add), [st4b], [st4b])
            V(lambda e: e.reciprocal(out=st4[:, 5, :], in_=st4[:, 4, :]), [st4b], [st4b])
            for j in range(4):
                for sl in range(2):
                    P(lambda e: e.transpose(out=T1[:, j * 2 + sl, :], in_=pb16[:, j, sl * 128:(sl + 1) * 128], identity=c.identH[:]), [pb16b, c.identH_b], [T1b])
            A(lambda e: e.copy(out=pTs[:], in_=T1[:]), [T1b], [pTsb])
            O4 = T2[:, 0:256].rearrange("p (h d) -> p h d", h=4)
            for j, h in enumerate(heads):
                kvh = h // 8
                for sl in range(2):
                    P(lambda e: e.matmul(O4[:, j, :], lhsT=pTs[:, j * 2 + sl, :], rhs=v2[:, sl, kvh * 64:(kvh + 1) * 64], start=(sl == 0), stop=(sl == 1)),
                      [pTsb, v2b], [T2b])
            V(lambda e: e.tensor_tensor(out=mixb[:, g4 * 256:(g4 + 1) * 256].rearrange("p (h d) -> p h d", h=4), in0=O4,
                                        in1=st4[:, 5, :].unsqueeze(2).to_broadcast([128, 4, 64]), op=ALU.mult), [T2b, st4b], [mixbb])
        kb.dma("sp", mix[(i - 1) * 128:i * 128, :], mixb[:], reads=[mixbb], writes=[mix_b])
    kb.pop()


def phase_outproj_ln_router(g, T, NE, get_xT_fn, xres, W, bias, mod, mod_b, lng_d, lnb_d, rw_d, rb_d,
                            h1, h1_b, xfT, xfT_b, gates, gates_b, wname):
    kb, V, A, P, G, c = g.kb, g.V, g.A, g.P, g.G, g.c
    NT = T // 128
    kb.push()
    lng, lnb_ = kb.sb("lng", [128, D], F32)
    lnbt, _ = kb.sb("lnb", [128, D], F32)
    kb.dma("sp", lng[:], lng_d.partition_broadcast(128), writes=[lnb_])
    kb.dma("sp", lnbt[:], lnb_d.partition_broadcast(128), writes=[lnb_])
    rw, rwb = kb.sb("rw", [128, 16, NE], F32)
    kb.dma("sp", rw[:], rw_d.rearrange("(kc p) n -> p kc n", p=128), writes=[rwb])
    rbt, rbb = kb.sb("rb", [1, NE], F32)
    kb.dma("sp", rbt[:], rb_d.rearrange("(o n) -> o n", o=1), writes=[rbb])
    xr, xrb = kb.sb("xres", [128, D], F32)
    t, tb = kb.sb("t", [128, D], F32)
    sq, sqb = kb.sb("sq", [128, D], F32)
    hh, hhb = sq, sqb
    xfb, xfbb = kb.sb("xfb", [128, D], BF16)
    xfTt, xfTb_ = kb.sb("xfTt", [128, 16, 128], BF16)
    st, stb = kb.sb("st", [128, 4], F32)
    lg, lgb = kb.sb("lg", [128, 3, NE], F32)
    m8, m8b = kb.sb("m8", [128, 8], F32)
    tps = [kb.ps("tps", [128, 8, 128], BF16)]
    tpf, tpfb = kb.ps("tpf", [128, 4, 128], F32)
    lps, lpsb = kb.ps("lps", [128, NE], F32)

    xfTf = xr[:].rearrange("p (k t) -> p k t", k=16)
    xfTfb = xrb

    def epi(ti, si, pm, pmb, c0, ncol):
        kb.dma("sp", xr[:], xres[ti * 128:(ti + 1) * 128, :], writes=[xrb])
        V(lambda e: e.tensor_tensor(out=t[:], in0=pm[:], in1=mod[:, 4096:6144], op=ALU.mult), [pmb, mod_b], [tb])
        V(lambda e: e.scalar_tensor_tensor(out=t[:], in0=xr[:], scalar=DN_ALPHA, in1=t[:], op0=ALU.mult, op1=ALU.add), [xrb, tb], [tb])
        emit_ln(g, t, tb, hh, hhb, lng, lnbt, lnb_, sq, sqb, st, stb)
        kb.dma("sp", h1[ti * 128:(ti + 1) * 128, :], hh[:], reads=[hhb], writes=[h1_b])
        V(lambda e: e.tensor_tensor(out=t[:], in0=hh[:], in1=mod[:, 8192:10240], op=ALU.mult), [hhb, mod_b], [tb])
        G(lambda e: e.tensor_tensor(out=t[:], in0=t[:], in1=mod[:, 6144:8192], op=ALU.add), [tb, mod_b], [tb])
        A(lambda e: e.copy(out=xfb[:], in_=t[:]), [tb], [xfbb])
        emit_transposes(g, xfb, xfbb, xfTt, xfTb_, tps)
        kb.dma("sp", xfT[:, :, ti * 128:(ti + 1) * 128], xfTt[:], reads=[xfTb_], writes=[xfT_b])
        for r in range(4):
            for j in range(4):
                kc = r * 4 + j
                P(lambda e: e.transpose(out=tpf[:, j, :], in_=t[:, kc * 128:(kc + 1) * 128], identity=c.identF[:]), [tb, c.identF_b], [tpfb])
            A(lambda e: e.copy(out=xfTf[:, r * 4:(r + 1) * 4, :], in_=tpf[:]), [tpfb], [xfTfb])
        for kc in range(16):
            P(lambda e: e.matmul(lps[:], lhsT=xfTf[:, kc, :], rhs=rw[:, kc, :], start=(kc == 0), stop=False), [xfTfb, rwb], [lpsb])
        P(lambda e: e.matmul(lps[:], lhsT=c.ones1[:], rhs=rbt[:], start=False, stop=True), [c.ones1_b, rbb], [lpsb])
        V(lambda e: e.tensor_copy(out=lg[:, 0, :], in_=lps[:]), [lpsb], [lgb])
        V(lambda e: e.max(out=m8[:], in_=lg[:, 0, :]), [lgb], [m8b])
        V(lambda e: e.tensor_scalar(out=lg[:, 1, :], in0=lg[:, 0, :], scalar1=m8[:, 3:4], scalar2=None, op0=ALU.is_ge), [lgb, m8b], [lgb])
        V(lambda e: e.tensor_scalar(out=lg[:, 0, :], in0=lg[:, 0, :], scalar1=m8[:, 0:1], scalar2=None, op0=ALU.subtract), [lgb, m8b], [lgb])
        A(lambda e: e.activation(out=lg[:, 0, :], in_=lg[:, 0, :], func=AF.Exp), [lgb], [lgb])
        V(lambda e: e.tensor_tensor(out=lg[:, 0, :], in0=lg[:, 0, :], in1=lg[:, 1, :], op=ALU.mult), [lgb], [lgb])
        V(lambda e: e.reduce_sum(out=m8[:, 4:5], in_=lg[:, 0, :], axis=AX.X), [lgb], [m8b])
        V(lambda e: e.reciprocal(out=m8[:, 5:6], in_=m8[:, 4:5]), [m8b], [m8b])
        V(lambda e: e.tensor_scalar(out=lg[:, 2, :], in0=lg[:, 0, :], scalar1=m8[:, 5:6], scalar2=None, op0=ALU.mult), [lgb, m8b], [lgb])
        kb.dma("sp", gates[ti * 128:(ti + 1) * 128, :], lg[:, 2, :], reads=[lgb], writes=[gates_b])

    linear_rows(g, NT, get_xT_fn, W, bias, [(0, 2048)], epi, wname)
    kb.pop()


def phase_moe(g, T, NE, xfT, xfT_b, gates, gates_b, w1, b1, w2, b2, acc_d, acc_b):
    kb, V, A, P, G, c = g.kb, g.V, g.A, g.P, g.G, g.c
    GT = min(1024, T)
    NG = T // GT
    NTG = GT // 128
    HW = min(512, GT)
    NH = GT // HW
    kb.push()
    b1T, b1Tb = kb.sb("b1T", [128, NE * 16], F32)
    kb.push()
    nrow = NE * 16
    for r0 in range(0, nrow, 128):
        rn = min(128, nrow - r0)
        rows, rb = kb.sb("b1rows", [128, 128], F32)
        kb.dma("sp", rows[0:rn, :], b1.rearrange("e (ch p) -> (e ch) p", p=128)[r0:r0 + rn, :], writes=[rb])
        pt, pb = kb.ps("b1ps", [128, 128], F32)
        P(lambda e: e.transpose(out=pt[:, 0:rn], in_=rows[0:rn, :], identity=c.identF[0:rn, 0:rn]), [rb, c.identF_b], [pb])
        V(lambda e: e.tensor_copy(out=b1T[:, r0:r0 + rn], in_=pt[:, 0:rn]), [pb], [b1Tb])
    kb.pop()
    xT, xTb = kb.sb("moe_xT", [128, 16, GT], BF16)
    acc, accb = kb.sb("moe_acc", [128, NTG, D], F32)
    actT, actTb = kb.sb("moe_act", [128, 8, GT], BF16)
    gt, gtb = kb.sb("moe_g", [128, NTG, NE], F32)
    w1s = [kb.sb("w1s", [128, 16, 512], BF16) for _ in range(2)]
    w2s = [kb.sb("w2s", [128, 8, 512], BF16) for _ in range(2)]
    b2s = [kb.sb("b2s", [1, D], F32)]
    gg, ggb = kb.sb("eg", [128, 512], F32)
    sgm, sgmb = kb.sb("esg", [128, 512], F32)
    ll, llb = kb.sb("el", [128, 512], F32)
    pg = [kb.ps("pg", [128, 512], F32) for _ in range(2)]
    pl = [kb.ps("pl", [128, 512], F32) for _ in range(2)]
    py = [kb.ps("py", [128, 512], F32) for _ in range(2)]
    n1 = n2 = 0
    for gi in range(NG):
        t0 = gi * GT
        kb.dma("sp", xT[:], xfT[:, :, t0:t0 + GT], reads=[xfT_b], writes=[xTb])
        kb.dma("sp", gt[:], gates[t0:t0 + GT, :].rearrange("(n p) e -> p n e", p=128), reads=[gates_b], writes=[gtb])
        V(lambda e: e.memset(acc[:], 0.0), [], [accb])
        for ex in range(NE):
            b2t, b2b = b2s[0]
            kb.dma("sp", b2t[:], b2[ex:ex + 1, :], writes=[b2b])
            for s in range(4):
                wt, wb = w1s[n1 % 2]
                n1 += 1
                kb.dma("pool", wt[:, :, 0:256], w1[ex, :, s * 256:(s + 1) * 256].rearrange("(kc p) n -> p kc n", p=128), writes=[wb])
                kb.dma("pool", wt[:, :, 256:512], w1[ex, :, 1024 + s * 256:1024 + (s + 1) * 256].rearrange("(kc p) n -> p kc n", p=128), writes=[wb])
                for j in range(2):
                    fc = s * 2 + j
                    for hf in range(NH):
                        pgt, pgb = pg[(j * NH + hf) % 2]
                        plt, plb = pl[(j * NH + hf) % 2]
                        ts = slice(hf * HW, (hf + 1) * HW)
                        for kc in range(16):
                            P(lambda e: e.matmul(pgt[:, 0:HW], lhsT=wt[:, kc, j * 128:(j + 1) * 128], rhs=xT[:, kc, ts], start=(kc == 0), stop=(kc == 15)), [wb, xTb], [pgb])
                        for kc in range(16):
                            P(lambda e: e.matmul(plt[:, 0:HW], lhsT=wt[:, kc, 256 + j * 128:256 + (j + 1) * 128], rhs=xT[:, kc, ts], start=(kc == 0), stop=(kc == 15)), [wb, xTb], [plb])
                        bg = b1T[:, ex * 16 + fc:ex * 16 + fc + 1]
                        bl = b1T[:, ex * 16 + 8 + fc:ex * 16 + 8 + fc + 1]
                        V(lambda e: e.tensor_scalar(out=gg[:, 0:HW], in0=pgt[:, 0:HW], scalar1=bg, scalar2=7.0, op0=ALU.add, op1=ALU.min), [pgb, b1Tb], [ggb])
                        A(lambda e: e.activation(out=sgm[:, 0:HW], in_=gg[:, 0:HW], func=AF.Sigmoid, scale=1.702), [ggb], [sgmb])
                        V(lambda e: e.tensor_scalar(out=ll[:, 0:HW], in0=plt[:, 0:HW], scalar1=bl, scalar2=7.0, op0=ALU.add, op1=ALU.min), [plb, b1Tb], [llb])
                        G(lambda e: e.tensor_scalar(out=ll[:, 0:HW], in0=ll[:, 0:HW], scalar1=-7.0, scalar2=1.0, op0=ALU.max, op1=ALU.add), [llb], [llb])
                        G(lambda e: e.tensor_tensor(out=gg[:, 0:HW], in0=gg[:, 0:HW], in1=sgm[:, 0:HW], op=ALU.mult), [ggb, sgmb], [ggb])
                        V(lambda e: e.tensor_tensor(out=actT[:, fc, ts], in0=gg[:, 0:HW], in1=ll[:, 0:HW], op=ALU.mult), [ggb, llb], [actTb])
            for s in range(4):
                wt, wb = w2s[n2 % 2]
                n2 += 1
                kb.dma("pool", wt[:], w2[ex, :, s * 512:(s + 1) * 512].rearrange("(kc p) n -> p kc n", p=128), writes=[wb])
                for ti in range(NTG):
                    pyt, pyb = py[ti % 2]
                    for kc in range(8):
                        P(lambda e: e.matmul(pyt[:], lhsT=actT[:, kc, ti * 128:(ti + 1) * 128], rhs=wt[:, kc, :], start=(kc == 0), stop=False), [actTb, wb], [pyb])
                    P(lambda e: e.matmul(pyt[:], lhsT=c.ones1[:], rhs=b2t[:, s * 512:(s + 1) * 512], start=False, stop=True), [c.ones1_b, b2b], [pyb])
                    V(lambda e: e.scalar_tensor_tensor(out=acc[:, ti, s * 512:(s + 1) * 512], in0=pyt[:], scalar=gt[:, ti, ex:ex + 1],
                                                       in1=acc[:, ti, s * 512:(s + 1) * 512], op0=ALU.mult, op1=ALU.add), [pyb, gtb, accb], [accb])
        for ti in range(NTG):
            kb.dma("sp", acc_d[t0 + ti * 128:t0 + (ti + 1) * 128, :], acc[:, ti, :], reads=[accb], writes=[acc_b])
    kb.pop()


def phase_ln2(g, T, h1, h1_b, acc_d, acc_b, mod, mod_b, lng_d, lnb_d, out_d, out_b):
    kb, V, G = g.kb, g.V, g.G
    NT = T // 128
    kb.push()
    lng, lnb_ = kb.sb("lng2", [128, D], F32)
    lnbt, _ = kb.sb("lnb2", [128, D], F32)
    kb.dma("sp", lng[:], lng_d.partition_broadcast(128), writes=[lnb_])
    kb.dma("sp", lnbt[:], lnb_d.partition_broadcast(128), writes=[lnb_])
    hs = [kb.sb("l2h", [128, D], F32) for _ in range(2)]
    as_ = [kb.sb("l2a", [128, D], F32) for _ in range(2)]
    os_ = [kb.sb("l2o", [128, D], F32) for _ in range(2)]
    sq, sqb = kb.sb("l2sq", [128, D], F32)
    st, stb = kb.sb("l2st", [128, 4], F32)
    for ti in range(NT):
        h, hb = hs[ti % 2]
        a, ab = as_[ti % 2]
        o, ob = os_[ti % 2]
        kb.dma("sp", h[:], h1[ti * 128:(ti + 1) * 128, :], reads=[h1_b], writes=[hb])
        kb.dma("sp", a[:], acc_d[ti * 128:(ti + 1) * 128, :], reads=[acc_b], writes=[ab])
        V(lambda e: e.tensor_tensor(out=a[:], in0=a[:], in1=mod[:, 10240:12288], op=ALU.mult), [ab, mod_b], [ab])
        V(lambda e: e.scalar_tensor_tensor(out=a[:], in0=h[:], scalar=DN_ALPHA, in1=a[:], op0=ALU.mult, op1=ALU.add), [hb, ab], [ab])
        emit_ln(g, a, ab, o, ob, lng, lnbt, lnb_, sq, sqb, st, stb)
        kb.dma("sp", out_d[ti * 128:(ti + 1) * 128, :], o[:], reads=[ob], writes=[out_b])
    kb.pop()


def layer0(g, T, NE, I, hout, hout_b):
    kb = g.kb
    kb.push()
    mod, mod_b = kb.sb("mod", [128, 6 * D], F32)
    compute_mod(g, I["c"], I["ada_w0"], I["ada_b0"], mod, mod_b)
    if getattr(g, "debug", False):
        md, md_b = scratch(g, "moddbg", [128, 6 * D], F32)
        kb.dma("sp", md, mod[:], reads=[mod_b], writes=[md_b])
    proj, proj_b = scratch(g, "proj", [T + 128, 5376], F32)
    mix, mix_b = scratch(g, "mix", [T, D], BF16)
    h1, h1_b = scratch(g, "h1", [T, D], F32)
    xfT, xfT_b = scratch(g, "xfT", [128, 16, T], BF16)
    gates, gates_b = scratch(g, "gates", [T, NE], F32)
    accd, accd_b = scratch(g, "accd", [T, D], F32)
    phase_inproj(g, T, I["xh"], I["ab_in_w"], I["ab_in_b"], mod, mod_b, proj, proj_b)
    phase_mixer(g, T, proj, proj_b, I["pos2d"], I["hv"], I["ab_sinks"], I["ab_gnorm_w"], I["lb_logits"], mix, mix_b)
    kb.push()
    mts = [kb.sb("mixt", [128, D], BF16)]
    tps = [kb.ps("tpsm", [128, 8, 128], BF16)]

    def get_xT(ti, xT, xTb):
        mt, mb = mts[0]
        kb.dma("sp", mt[:], mix[ti * 128:(ti + 1) * 128, :], reads=[mix_b], writes=[mb])
        emit_transposes(g, mt, mb, xT, xTb, tps)
    phase_outproj_ln_router(g, T, NE, get_xT, I["xh"][128:, :], I["ab_out_w"], I["ab_out_b"], mod, mod_b,
                            I["ln_g00"], I["ln_b00"], I["router_w0"], I["router_b0"], h1, h1_b, xfT, xfT_b, gates, gates_b, "w_out")
    kb.pop()
    modrow, modrow_b = scratch(g, "modrow0", [6 * D], F32)
    for q in range(6):
        kb.dma("sp", modrow[q * D:(q + 1) * D].rearrange("(o n) -> o n", o=1), mod[0:1, q * D:(q + 1) * D], reads=[mod_b], writes=[modrow_b])
    kb.pop()
    phase_moe(g, T, NE, xfT, xfT_b, gates, gates_b, I["exp_w1_0"], I["exp_b1_0"], I["exp_w2_0"], I["exp_b2_0"], accd, accd_b)
    kb.push()
    mod, mod_b = kb.sb("mod0b", [128, 6 * D], F32)
    for q in range(6):
        kb.dma("sp", mod[:, q * D:(q + 1) * D], modrow[q * D:(q + 1) * D].partition_broadcast(128), reads=[modrow_b], writes=[mod_b])
    phase_ln2(g, T, h1, h1_b, accd, accd_b, mod, mod_b, I["ln_g01"], I["ln_b01"], hout, hout_b)
    kb.pop()


def l0_input_specs(T, NE):
    sp = {
        "xh": ([T + 128, D], F32), "pos2d": ([T // 128 + 1, 128], I32), "hv": ([128, 1], F32), "c": ([D], F32),
        "ada_w0": ([D, 6 * D], F32), "ada_b0": ([6 * D], F32), "ab_in_w": ([D, 5376], F32), "ab_in_b": ([5376], F32),
        "ab_sinks": ([16], F32), "ab_gnorm_w": ([8, 128], F32), "lb_logits": ([3, 1024], F32),
        "ab_out_w": ([D, D], F32), "ab_out_b": ([D], F32),
        "ln_g00": ([D], F32), "ln_b00": ([D], F32), "ln_g01": ([D], F32), "ln_b01": ([D], F32),
        "router_w0": ([D, NE], F32), "router_b0": ([NE], F32),
        "exp_w1_0": ([NE, D, D], F32), "exp_b1_0": ([NE, D], F32), "exp_w2_0": ([NE, D // 2, D], F32), "exp_b2_0": ([NE, D], F32),
    }
    for n, shp in CONST_SHAPES.items():
        sp[n] = (shp, F32)
    return sp


def build_l0(T, NE, debug=False):
    g = mk_ctx(l0_input_specs(T, NE), {"hmid": ([T, D], F32)})
    g.debug = debug
    load_consts(g)
    ob = g.kb.buf("hmid")
    layer0(g, T, NE, g.din, g.dout["hmid"], ob)
    g.kb.close()
    return g.nc


def shard_l0(inp, B, S, NCORES, NE):
    cpb = NCORES // B
    T = S // cpb
    consts = host_consts()
    maps = []
    for core in range(NCORES):
        b, k = core // cpb, core % cpb
        s0 = k * T
        xh = np.zeros((T + 128, D), np.float32)
        pos = np.zeros((T + 128,), np.int32)
        if k > 0:
            xh[:] = inp["x"][b, s0 - 128:s0 + T]
            pos[:] = inp["positions"][b, s0 - 128:s0 + T]
        else:
            xh[128:] = inp["x"][b, s0:s0 + T]
            pos[128:] = inp["positions"][b, s0:s0 + T]
        m = {
            "xh": xh, "pos2d": pos.reshape(-1, 128), "hv": np.full((128, 1), 1.0 if k > 0 else 0.0, np.float32),
            "c": inp["c"][b], "ada_w0": inp["ada_w"][0], "ada_b0": inp["ada_b"][0],
            "ab_in_w": inp["ab_in_w"][0], "ab_in_b": inp["ab_in_b"][0], "ab_sinks": inp["ab_sinks"][0],
            "ab_gnorm_w": inp["ab_gnorm_w"][0], "lb_logits": inp["hgrn_lb_logits"],
            "ab_out_w": inp["ab_out_w"][0], "ab_out_b": inp["ab_out_b"][0],
            "ln_g00": inp["ln_g"][0, 0], "ln_b00": inp["ln_b"][0, 0], "ln_g01": inp["ln_g"][0, 1], "ln_b01": inp["ln_b"][0, 1],
            "router_w0": inp["router_w"][0], "router_b0": inp["router_b"][0],
            "exp_w1_0": inp["exp_w1"][0], "exp_b1_0": inp["exp_b1"][0], "exp_w2_0": inp["exp_w2"][0], "exp_b2_0": inp["exp_b2"][0],
        }
        m.update(consts)
        maps.append({k_: np.ascontiguousarray(v) for k_, v in m.items()})
    return maps, T


def linear_cols(g, T, get_rhs, W, epilogue, wname):
    kb, P = g.kb, g.P
    kb.push()
    wt, wb = kb.sb(wname, [128, 16, D], BF16)
    for q in range(4):
        kb.dma("pool", wt[:, :, q * 512:(q + 1) * 512], W[:, q * 512:(q + 1) * 512].rearrange("(kc p) n -> p kc n", p=128), writes=[wb])
    GW = min(512, T)
    ps = [kb.ps("lc_ps", [128, 512], F32) for _ in range(2)]
    for gi in range(T // GW):
        rhs, rhsb = get_rhs(gi, GW)
        for m in range(16):
            pt, pb = ps[m % 2]
            for kc in range(16):
                P(lambda e: e.matmul(pt[:, 0:GW], lhsT=wt[:, kc, m * 128:(m + 1) * 128], rhs=rhs[:, kc, :], start=(kc == 0), stop=(kc == 15)), [wb, rhsb], [pb])
            epilogue(gi, m, pt, pb, GW)
    kb.pop()


def phase_uT(g, T, hin, hin_b, W, mod, mod_b, uT, uT_b):
    kb, V, A, G = g.kb, g.V, g.A, g.G
    kb.push()
    xs = [kb.sb("ux", [128, D], F32) for _ in range(2)]
    xq, xqb = kb.sb("uxq", [128, D], BF16)
    xTg = [kb.sb("uxT", [128, 16, min(512, T)], BF16) for _ in range(2)]
    tps = [kb.ps("utps", [128, 8, 128], BF16) for _ in range(2)]
    ust = [kb.sb("ust", [128, 512], F32) for _ in range(2)]
    cnt = [0]

    def get_rhs(gi, GW):
        xT, xTb = xTg[gi % 2]
        for j in range(GW // 128):
            ti = gi * (GW // 128) + j
            x, xb = xs[ti % 2]
            kb.dma("sp", x[:], hin[ti * 128:(ti + 1) * 128, :], reads=[hin_b], writes=[xb])
            V(lambda e: e.tensor_tensor(out=x[:], in0=x[:], in1=mod[:, 2048:4096], op=ALU.mult), [xb, mod_b], [xb])
            G(lambda e: e.tensor_tensor(out=xq[:], in0=x[:], in1=mod[:, 0:2048], op=ALU.add), [xb, mod_b], [xqb])
            emit_transposes(g, xq, xqb, xT[:, :, j * 128:(j + 1) * 128], xTb, tps)
        return xT, xTb

    def epi(gi, m, pt, pb, GW):
        u, ub = ust[cnt[0] % 2]
        cnt[0] += 1
        A(lambda e: e.copy(out=u[:, 0:GW], in_=pt[:, 0:GW]), [pb], [ub])
        kb.dma("sp", uT[:, m, gi * GW:(gi + 1) * GW], u[:, 0:GW], reads=[ub], writes=[uT_b])

    linear_cols(g, T, get_rhs, W, epi, "w_cin")
    kb.pop()


def s5_setup(g, I, T, need_pow):
    kb, V, A, P, G, c = g.kb, g.V, g.A, g.P, g.G, g.c
    L = LSUB
    t = Ctx()
    t.Eb = kb.buf("Etab")
    t.ER, _ = kb.sb("ER", [128, 64, L], F32)
    t.EI, _ = kb.sb("EI", [128, 64, L], F32)
    t.EiR, _ = kb.sb("EiR", [128, 64, L], F32)
    t.EiI, _ = kb.sb("EiI", [128, 64, L], F32)
    t.Wb = kb.buf("Wtab")
    t.WBR, _ = kb.sb("WBR", [128, 32, 128], BF16)
    t.WBI, _ = kb.sb("WBI", [128, 32, 128], BF16)
    t.WCR, _ = kb.sb("WCR", [128, 64, 64], F32)
    t.WCI, _ = kb.sb("WCI", [128, 64, 64], F32)
    t.Dcol, t.Dcol_b = colvec(g, I["c_D"].rearrange("g c -> (g c)"), "Dcol")
    t.rmask, t.rmask_b = kb.sb("rmask", [128, 64 * L], F32)
    kb.dma("sp", t.rmask[:], g.din["rmask"].rearrange("o n -> (o n)").partition_broadcast(128), writes=[t.rmask_b])
    if need_pow:
        t.PR, t.Pb = kb.sb("PR", [128, 64], F32)
        t.PI, _ = kb.sb("PI", [128, 64], F32)
    kb.push()
    ar, arb = kb.sb("ar", [64, 128], F32)
    ai, aib = kb.sb("ai", [64, 128], F32)
    kb.dma("sp", ar[:], I["c_A_re"].rearrange("g p -> (g p)").rearrange("(b m) -> b m", m=128), writes=[arb])
    kb.dma("sp", ai[:], I["c_A_im"].rearrange("g p -> (g p)").rearrange("(b m) -> b m", m=128), writes=[aib])
    ld2, ld2b = kb.sb("ld2", [64, 2], F32)
    kb.dma("sp", ld2[:], I["c_log_dt"].rearrange("(b two) -> b two", two=2), writes=[ld2b])
    A(lambda e: e.activation(out=ld2[:], in_=ld2[:], func=AF.Exp), [ld2b], [ld2b])
    dtb, dtbb = kb.sb("dtb", [64, 128], F32)
    for hf in range(2):
        V(lambda e: e.tensor_copy(out=dtb[:, hf * 64:(hf + 1) * 64], in_=ld2[:, hf:hf + 1].to_broadcast([64, 64])), [ld2b], [dtbb])
    lr, lrb = kb.sb("lr", [64, 128], F32)
    li, lib = kb.sb("li", [64, 128], F32)
    V(lambda e: e.tensor_tensor(out=lr[:], in0=ar[:], in1=dtb[:], op=ALU.mult), [arb, dtbb], [lrb])
    V(lambda e: e.tensor_tensor(out=li[:], in0=ai[:], in1=dtb[:], op=ALU.mult), [aib, dtbb], [lib])
    tp, tpb = kb.ps("s5tp", [128, 128], F32)
    outs = []
    for nm, (src, srcb) in (("lrT", (lr, lrb)), ("liT", (li, lib)), ("arT", (ar, arb)), ("aiT", (ai, aib))):
        o, ob = kb.sb(nm, [128, 64], F32)
        P(lambda e: e.transpose(out=tp[:, 0:64], in_=src[:], identity=c.identF[0:64, 0:64]), [srcb, c.identF_b], [tpb])
        V(lambda e: e.tensor_copy(out=o[:], in_=tp[:, 0:64]), [tpb], [ob])
        outs.append((o, ob))
    (lrT, lrTb), (liT, liTb), (arT, arTb), (aiT, aiTb) = outs
    arg, argb = kb.sb("arg", [128, 64, L], F32)
    mag, magb = kb.sb("mag", [128, 64, L], F32)
    for t_ in range(L):
        V(lambda e: e.tensor_scalar(out=arg[:, :, t_], in0=liT[:], scalar1=float(t_ + 1), scalar2=None, op0=ALU.mult), [liTb], [argb])
        G(lambda e: e.tensor_scalar(out=mag[:, :, t_], in0=lrT[:], scalar1=float(t_ + 1), scalar2=None, op0=ALU.mult), [lrTb], [magb])
    sn, snb = kb.sb("sn", [128, 64, L], F32)
    cs, _ = kb.sb("cs", [128, 64, L], F32)
    emit_sincos(g, arg, argb, sn[:], cs[:], snb, [128, 64, L], "s5")
    em, emb = kb.sb("em", [128, 64, L], F32)
    A(lambda e: e.activation(out=em[:], in_=mag[:], func=AF.Exp), [magb], [emb])
    V(lambda e: e.tensor_tensor(out=t.ER[:], in0=em[:], in1=cs[:], op=ALU.mult), [emb, snb], [t.Eb])
    V(lambda e: e.tensor_tensor(out=t.EI[:], in0=em[:], in1=sn[:], op=ALU.mult), [emb, snb], [t.Eb])
    A(lambda e: e.activation(out=em[:], in_=mag[:], func=AF.Exp, scale=-1.0), [magb, t.Eb], [emb])
    V(lambda e: e.tensor_tensor(out=t.EiR[:], in0=em[:], in1=cs[:], op=ALU.mult), [emb, snb], [t.Eb])
    V(lambda e: e.scalar_tensor_tensor(out=t.EiI[:], in0=em[:], scalar=-1.0, in1=sn[:], op0=ALU.mult, op1=ALU.mult), [emb, snb], [t.Eb])
    w6, w6b = kb.sb("w6", [128, 8, 64], F32)
    nr, ni, t0, t1, fr, fi = [w6[:, j, :] for j in range(6)]
    V(lambda e: e.tensor_scalar(out=nr, in0=t.ER[:, :, 0], scalar1=-1.0, scalar2=None, op0=ALU.add), [t.Eb], [w6b])
    V(lambda e: e.tensor_copy(out=ni, in_=t.EI[:, :, 0]), [t.Eb], [w6b])
    V(lambda e: e.tensor_tensor(out=t0, in0=arT[:], in1=arT[:], op=ALU.mult), [arTb], [w6b])
    V(lambda e: e.tensor_tensor(out=t1, in0=aiT[:], in1=aiT[:], op=ALU.mult), [aiTb], [w6b])
    V(lambda e: e.tensor_tensor(out=t0, in0=t0, in1=t1, op=ALU.add), [w6b], [w6b])
    V(lambda e: e.reciprocal(out=w6[:, 6, :], in_=t0), [w6b], [w6b])
    V(lambda e: e.tensor_tensor(out=t0, in0=nr, in1=arT[:], op=ALU.mult), [w6b, arTb], [w6b])
    V(lambda e: e.tensor_tensor(out=t1, in0=ni, in1=aiT[:], op=ALU.mult), [w6b, aiTb], [w6b])
    V(lambda e: e.tensor_tensor(out=t0, in0=t0, in1=t1, op=ALU.add), [w6b], [w6b])
    V(lambda e: e.tensor_tensor(out=fr, in0=t0, in1=w6[:, 6, :], op=ALU.mult), [w6b], [w6b])
    V(lambda e: e.tensor_tensor(out=t0, in0=ni, in1=arT[:], op=ALU.mult), [w6b, arTb], [w6b])
    V(lambda e: e.tensor_tensor(out=t1, in0=nr, in1=aiT[:], op=ALU.mult), [w6b, aiTb], [w6b])
    V(lambda e: e.tensor_tensor(out=t0, in0=t0, in1=t1, op=ALU.subtract), [w6b], [w6b])
    V(lambda e: e.tensor_tensor(out=fi, in0=t0, in1=w6[:, 6, :], op=ALU.mult), [w6b], [w6b])
    Br, Brb = kb.sb("Br", [128, 64, 16], F32)
    Bi, Bib = kb.sb("Bi", [128, 64, 16], F32)
    for (dst, dstb, src) in ((Br, Brb, I["c_B_re"]), (Bi, Bib, I["c_B_im"])):
        v = src.rearrange("g p c -> (g p) c").rearrange("(b m) c -> m b c", m=128)
        for q in range(8):
            kb.dma("sp", dst[:, q * 8:(q + 1) * 8, :], v[:, q * 8:(q + 1) * 8, :], writes=[dstb])
    X1, X1b = kb.sb("X1", [128, 64, 16], F32)
    X2, X2b = kb.sb("X2", [128, 64, 16], F32)
    Bbr, Bbrb = kb.sb("Bbr", [128, 64, 16], F32)
    Bbi, Bbib = kb.sb("Bbi", [128, 64, 16], F32)
    frb = fr.unsqueeze(2).to_broadcast([128, 64, 16])
    fib = fi.unsqueeze(2).to_broadcast([128, 64, 16])
    V(lambda e: e.tensor_tensor(out=X1[:], in0=Br[:], in1=frb, op=ALU.mult), [Brb, w6b], [X1b])
    V(lambda e: e.tensor_tensor(out=X2[:], in0=Bi[:], in1=fib, op=ALU.mult), [Bib, w6b], [X2b])
    V(lambda e: e.tensor_tensor(out=Bbr[:], in0=X1[:], in1=X2[:], op=ALU.subtract), [X1b, X2b], [Bbrb])
    V(lambda e: e.tensor_tensor(out=X1[:], in0=Bi[:], in1=frb, op=ALU.mult), [Bib, w6b], [X1b])
    V(lambda e: e.tensor_tensor(out=X2[:], in0=Br[:], in1=fib, op=ALU.mult), [Brb, w6b], [X2b])
    V(lambda e: e.tensor_tensor(out=Bbi[:], in0=X1[:], in1=X2[:], op=ALU.add), [X1b, X2b], [Bbib])
    Q, Qb = kb.sb("Q", [128, 64, 2, 16], F32)
    for (srcB, srcBb, dstW) in ((Bbr, Bbrb, t.WBR), (Bbi, Bbib, t.WBI)):
        for gg in range(2):
            V(lambda e: e.tensor_scalar(out=Q[:, :, gg, :], in0=srcB[:], scalar1=c.hmask[:, gg:gg + 1], scalar2=None, op0=ALU.mult), [srcBb, c.hmask_b], [Qb])
        for cc in range(16):
            P(lambda e: e.transpose(out=tp[:], in_=Q[:, cc * 4:(cc + 1) * 4, :, :].rearrange("p b g c -> p (b g c)"), identity=c.identF[:]), [Qb, c.identF_b], [tpb])
            for jj in range(2):
                V(lambda e: e.tensor_scalar(out=dstW[:, cc * 2 + jj, :], in0=tp[:], scalar1=c.hmask[:, 4 + jj:5 + jj], scalar2=None, op0=ALU.mult), [tpb, c.hmask_b], [t.Wb])
    V(lambda e: e.memset(t.WCR[:], 0.0), [], [t.Wb])
    V(lambda e: e.memset(t.WCI[:], 0.0), [], [t.Wb])
    Cr, Crb = kb.sb("Cr", [128, 16, 64], F32)
    Cin, Cinb = kb.sb("Cin", [128, 16, 2, 64], F32)
    for (src, dstW, sgn) in ((I["c_C_re"], t.WCR, 1.0), (I["c_C_im"], t.WCI, -1.0)):
        kb.dma("sp", Cr[:], src.rearrange("g c p -> (g c) p").rearrange("(cc r) p -> r cc p", r=128), writes=[Crb])
        for gg in range(2):
            V(lambda e: e.tensor_scalar(out=Cin[:, :, gg, :], in0=Cr[:], scalar1=c.hmask[:, 2 + gg:3 + gg], scalar2=sgn, op0=ALU.mult, op1=ALU.mult), [Crb, c.hmask_b], [Cinb])
        for cc in range(16):
            P(lambda e: e.transpose(out=tp[:], in_=Cin[:, cc, :, :].rearrange("p g q -> p (g q)"), identity=c.identF[:]), [Cinb, c.identF_b], [tpb])
            for j in range(4):
                A(lambda e: e.copy(out=dstW[:, cc * 4 + j, (j % 2) * 32:(j % 2) * 32 + 32], in_=tp[:, 32 * j:32 * j + 32]), [tpb], [t.Wb])
    if need_pow:
        n2 = (T // L).bit_length() - 1
        assert (1 << n2) == T // L
        V(lambda e: e.tensor_copy(out=t.PR[:], in_=t.ER[:, :, L - 1]), [t.Eb], [t.Pb])
        V(lambda e: e.tensor_copy(out=t.PI[:], in_=t.EI[:, :, L - 1]), [t.Eb], [t.Pb])
        for _ in range(n2):
            V(lambda e: e.tensor_tensor(out=t0, in0=t.PR[:], in1=t.PR[:], op=ALU.mult), [t.Pb], [w6b])
            V(lambda e: e.tensor_tensor(out=t1, in0=t.PI[:], in1=t.PI[:], op=ALU.mult), [t.Pb], [w6b])
            V(lambda e: e.scalar_tensor_tensor(out=t.PI[:], in0=t.PR[:], scalar=2.0, in1=t.PI[:], op0=ALU.mult, op1=ALU.mult), [t.Pb], [t.Pb])
            V(lambda e: e.tensor_tensor(out=t.PR[:], in0=t0, in1=t1, op=ALU.subtract), [w6b], [t.Pb])
    kb.pop()
    return t


def s5_scan(g, T, tb, uT, uT_b, x0, emit_y, ygT, ygT_b, xend, xend_b):
    kb, V, A, P, G, c = g.kb, g.V, g.A, g.P, g.G, g.c
    L = LSUB
    NT = T // 128
    kb.push()
    names = ["BR", "BI", "t1", "t2", "t3", "t4", "XR", "XI"]
    bufs = {n: kb.sb("s5" + n, [128, 64, L], F32) for n in names}
    BR, BRb = bufs["BR"]; BI, BIb = bufs["BI"]
    t1, t1b = bufs["t1"]; t2, t2b = bufs["t2"]; t3, t3b = bufs["t3"]; t4, t4b = bufs["t4"]
    wr, wrb = BR, BRb
    wi, wib = BI, BIb
    XR, XRb = bufs["XR"]; XI, XIb = bufs["XI"]
    uts = [kb.sb("s5u", [128, 16, 128], F32)]
    uh, uhb = kb.sb("s5uh", [128, 16, 128], BF16)
    ygs, ygsb = kb.sb("s5yg", [128, 16, 128], BF16)
    yv, yvb = kb.sb("s5yv", [128, 16, L], F32)
    y2, y2b = kb.sb("s5y2", [128, 16, L], F32)
    pR = [kb.ps("s5pR", [128, 16, L], F32) for _ in range(2)]
    pI = [kb.ps("s5pI", [128, 16, L], F32) for _ in range(2)]
    pY, pYb = kb.ps("s5pY", [128, 16, L], F32)
    if x0 is None:
        V(lambda e: e.memset(XR[:], 0.0), [], [XRb])
        V(lambda e: e.memset(XI[:], 0.0), [], [XIb])
    else:
        V(lambda e: e.tensor_copy(out=XR[:, :, L - 1], in_=x0[0]), [x0[2]], [XRb])
        V(lambda e: e.tensor_copy(out=XI[:, :, L - 1], in_=x0[1]), [x0[2]], [XIb])
    flat = lambda a: a[:].rearrange("p b t -> p (b t)")
    for ti in range(NT):
        ut, utb = uts[0]
        kb.dma("sp", ut[:], uT[:, :, ti * 128:(ti + 1) * 128], reads=[uT_b], writes=[utb])
        A(lambda e: e.copy(out=uh[:], in_=ut[:]), [utb], [uhb])
        for sub in range(128 // L):
            ts = slice(sub * L, (sub + 1) * L)
            BR4 = BR[:].rearrange("p (cc j) t -> p cc j t", j=4)
            BI4 = BI[:].rearrange("p (cc j) t -> p cc j t", j=4)
            for qq in range(2):
                for half in range(2):
                    pr, prb = pR[half]
                    pi_, pib = pI[half]
                    hb_ = half * 64
                    for c8 in range(8):
                        cc = qq * 8 + c8
                        for jj in range(2):
                            sl_ = c8 * 2 + jj
                            P(lambda e: e.matmul(pr[:, sl_, :], lhsT=tb.WBR[hb_:hb_ + 64, cc * 2 + jj, :], rhs=uh[hb_:hb_ + 64, cc, ts], start=True, stop=True), [tb.Wb, uhb], [prb])
                            P(lambda e: e.matmul(pi_[:, sl_, :], lhsT=tb.WBI[hb_:hb_ + 64, cc * 2 + jj, :], rhs=uh[hb_:hb_ + 64, cc, ts], start=True, stop=True), [tb.Wb, uhb], [pib])
                for half in range(2):
                    pr, prb = pR[half]
                    pi_, pib = pI[half]
                    A(lambda e: e.copy(out=BR4[:, qq * 8:(qq + 1) * 8, half * 2:half * 2 + 2, :], in_=pr[:].rearrange("p (c j) t -> p c j t", j=2)), [prb], [BRb])
                    A(lambda e: e.copy(out=BI4[:, qq * 8:(qq + 1) * 8, half * 2:half * 2 + 2, :], in_=pi_[:].rearrange("p (c j) t -> p c j t", j=2)), [pib], [BIb])
            V(lambda e: e.tensor_tensor(out=t1[:], in0=BR[:], in1=tb.EiR[:], op=ALU.mult), [BRb, tb.Eb], [t1b])
            G(lambda e: e.tensor_tensor(out=t2[:], in0=BI[:], in1=tb.EiI[:], op=ALU.mult), [BIb, tb.Eb], [t2b])
            V(lambda e: e.tensor_tensor(out=t1[:], in0=t1[:], in1=t2[:], op=ALU.subtract), [t1b, t2b], [t1b])
            G(lambda e: e.tensor_tensor(out=t3[:], in0=BR[:], in1=tb.EiI[:], op=ALU.mult), [BRb, tb.Eb], [t3b])
            V(lambda e: e.tensor_tensor(out=t4[:], in0=BI[:], in1=tb.EiR[:], op=ALU.mult), [BIb, tb.Eb], [t4b])
            V(lambda e: e.tensor_tensor(out=t3[:], in0=t3[:], in1=t4[:], op=ALU.add), [t3b, t4b], [t3b])
            V(lambda e: e.tensor_tensor(out=t1[:, :, 0], in0=t1[:, :, 0], in1=XR[:, :, L - 1], op=ALU.add), [t1b, XRb], [t1b])
            V(lambda e: e.tensor_tensor(out=t3[:, :, 0], in0=t3[:, :, 0], in1=XI[:, :, L - 1], op=ALU.add), [t3b, XIb], [t3b])
            V(lambda e: e.tensor_tensor_scan(out=flat(wr), data0=tb.rmask[:], data1=flat(t1), initial=0.0, op0=ALU.mult, op1=ALU.add), [t1b, tb.rmask_b], [wrb])
            V(lambda e: e.tensor_tensor_scan(out=flat(wi), data0=tb.rmask[:], data1=flat(t3), initial=0.0, op0=ALU.mult, op1=ALU.add), [t3b, tb.rmask_b], [wib])
            V(lambda e: e.tensor_tensor(out=t2[:], in0=wr[:], in1=tb.ER[:], op=ALU.mult), [wrb, tb.Eb], [t2b])
            G(lambda e: e.tensor_tensor(out=t4[:], in0=wi[:], in1=tb.EI[:], op=ALU.mult), [wib, tb.Eb], [t4b])
            V(lambda e: e.tensor_tensor(out=XR[:], in0=t2[:], in1=t4[:], op=ALU.subtract), [t2b, t4b], [XRb])
            G(lambda e: e.tensor_tensor(out=t2[:], in0=wr[:], in1=tb.EI[:], op=ALU.mult), [wrb, tb.Eb], [t2b])
            V(lambda e: e.tensor_tensor(out=t4[:], in0=wi[:], in1=tb.ER[:], op=ALU.mult), [wib, tb.Eb], [t4b])
            V(lambda e: e.tensor_tensor(out=XI[:], in0=t2[:], in1=t4[:], op=ALU.add), [t2b, t4b], [XIb])
            if not emit_y:
                continue
            for b in range(64):
                cc, j = b // 4, b % 4
                hb_ = (j // 2) * 64
                P(lambda e: e.matmul(pY[hb_:hb_ + 64, cc, :], lhsT=tb.WCR[:, b, :], rhs=XR[:, b, :], start=(j % 2 == 0), stop=False), [tb.Wb, XRb], [pYb])
                P(lambda e: e.matmul(pY[hb_:hb_ + 64, cc, :], lhsT=tb.WCI[:, b, :], rhs=XI[:, b, :], start=False, stop=(j % 2 == 1)), [tb.Wb, XIb], [pYb])
            V(lambda e: e.tensor_tensor(out=yv[:], in0=ut[:, :, ts], in1=tb.Dcol[:].unsqueeze(2).to_broadcast([128, 16, L]), op=ALU.mult), [utb, tb.Dcol_b], [yvb])
            V(lambda e: e.tensor_tensor(out=yv[:], in0=yv[:], in1=pY[:], op=ALU.add), [yvb, pYb], [yvb])
            G(lambda e: e.tensor_tensor(out=y2[:], in0=yv[:], in1=yv[:], op=ALU.mult), [yvb], [y2b])
            V(lambda e: e.tensor_scalar(out=y2[:], in0=y2[:], scalar1=0.044715, scalar2=1.0, op0=ALU.mult, op1=ALU.add), [y2b], [y2b])
            V(lambda e: e.tensor_tensor(out=y2[:], in0=y2[:], in1=yv[:], op=ALU.mult), [y2b, yvb], [y2b])
            A(lambda e: e.activation(out=y2[:], in_=y2[:], func=AF.Sigmoid, scale=1.5957691216057308), [y2b], [y2b])
            V(lambda e: e.tensor_tensor(out=ygs[:, :, ts], in0=yv[:], in1=y2[:], op=ALU.mult), [yvb, y2b], [ygsb])
        if emit_y:
            kb.dma("sp", ygT[:, :, ti * 128:(ti + 1) * 128], ygs[:], reads=[ygsb], writes=[ygT_b])
    xe, xeb = kb.sb("s5xe", [128, 64, 2], F32)
    V(lambda e: e.tensor_copy(out=xe[:, :, 0], in_=XR[:, :, L - 1]), [XRb], [xeb])
    V(lambda e: e.tensor_copy(out=xe[:, :, 1], in_=XI[:, :, L - 1]), [XIb], [xeb])
    kb.dma("sp", xend, xe[:], reads=[xeb], writes=[xend_b])
    kb.pop()


def phase_glu(g, T, ygT, ygT_b, W, glub_d, zT, zT_b):
    kb, V, A = g.kb, g.V, g.A
    kb.push()
    glub, glubb = colvec(g, glub_d, "glub")
    GWm = min(512, T)
    ygs = [kb.sb("gyg", [128, 16, GWm], BF16) for _ in range(2)]
    zs, zsb = kb.sb("gz", [128, 16, GWm], BF16)
    sg, sgb = kb.sb("gsg", [128, 512], F32)
    cur = [None]

    def get_rhs(gi, GW):
        y, yb = ygs[gi % 2]
        kb.dma("sp", y[:], ygT[:, :, gi * GW:(gi + 1) * GW], reads=[ygT_b], writes=[yb])
        cur[0] = (y, yb)
        return y, yb

    def epi(gi, m, pt, pb, GW):
        y, yb = cur[0]
        A(lambda e: e.activation(out=sg[:, 0:GW], in_=pt[:, 0:GW], func=AF.Sigmoid, bias=glub[:, m:m + 1]), [pb, glubb], [sgb])
        V(lambda e: e.tensor_tensor(out=zs[:, m, :], in0=sg[:, 0:GW], in1=y[:, m, :], op=ALU.mult), [sgb, yb], [zsb])
        if m == 15:
            kb.dma("sp", zT[:, :, gi * GW:(gi + 1) * GW], zs[:], reads=[zsb], writes=[zT_b])

    linear_cols(g, T, get_rhs, W, epi, "w_glu")
    kb.pop()


def layer1_front(g, T, I, hin, hin_b):
    kb = g.kb
    modrow = scratch(g, "modrow1", [6 * D], F32)
    uT = scratch(g, "uT", [128, 16, T], F32)
    kb.push()
    mod, mod_b = kb.sb("mod1", [128, 6 * D], F32)
    compute_mod(g, I["c"], I["ada_w1"], I["ada_b1"], mod, mod_b)
    for q in range(6):
        kb.dma("sp", modrow[0][q * D:(q + 1) * D].rearrange("(o n) -> o n", o=1), mod[0:1, q * D:(q + 1) * D], reads=[mod_b], writes=[modrow[1]])
    phase_uT(g, T, hin, hin_b, I["c_in_w"], mod, mod_b, uT[0], uT[1])
    kb.pop()
    return modrow, uT


def layer1_back(g, T, NE, I, modrow, uT, hin, hin_b, xe_prev, out_d, out_b):
    kb, V = g.kb, g.V
    ygT = scratch(g, "ygT", [128, 16, T], BF16)
    zT = scratch(g, "zT", [128, 16, T], BF16)
    h1 = scratch(g, "h1b", [T, D], F32)
    xfT = scratch(g, "xfTb", [128, 16, T], BF16)
    gates = scratch(g, "gatesb", [T, NE], F32)
    accd = scratch(g, "accdb", [T, D], F32)
    xdummy = scratch(g, "xdummy", [128, 64, 2], F32)
    kb.push()
    tb = s5_setup(g, I, T, True)
    xp, xpb = kb.sb("xp", [128, 3, 64, 2], F32)
    for d_ in range(3):
        kb.dma("sp", xp[:, d_, :, :], xe_prev[d_], writes=[xpb])
    x0, x0b = kb.sb("x0", [128, 4, 64], F32)
    ar_, ai_, t0, t1 = [x0[:, j, :] for j in range(4)]
    V(lambda e: e.tensor_copy(out=ar_, in_=xp[:, 2, :, 0]), [xpb], [x0b])
    V(lambda e: e.tensor_copy(out=ai_, in_=xp[:, 2, :, 1]), [xpb], [x0b])
    for d_ in (1, 0):
        V(lambda e: e.tensor_tensor(out=t0, in0=ar_, in1=tb.PR[:], op=ALU.mult), [x0b, tb.Pb], [x0b])
        V(lambda e: e.tensor_tensor(out=t1, in0=ai_, in1=tb.PI[:], op=ALU.mult), [x0b, tb.Pb], [x0b])
        V(lambda e: e.tensor_tensor(out=t0, in0=t0, in1=t1, op=ALU.subtract), [x0b], [x0b])
        V(lambda e: e.tensor_tensor(out=t1, in0=ar_, in1=tb.PI[:], op=ALU.mult), [x0b, tb.Pb], [x0b])
        V(lambda e: e.tensor_tensor(out=ai_, in0=ai_, in1=tb.PR[:], op=ALU.mult), [x0b, tb.Pb], [x0b])
        V(lambda e: e.tensor_tensor(out=ai_, in0=ai_, in1=t1, op=ALU.add), [x0b], [x0b])
        V(lambda e: e.tensor_tensor(out=ar_, in0=t0, in1=xp[:, d_, :, 0], op=ALU.add), [x0b, xpb], [x0b])
        V(lambda e: e.tensor_tensor(out=ai_, in0=ai_, in1=xp[:, d_, :, 1], op=ALU.add), [x0b, xpb], [x0b])
    s5_scan(g, T, tb, uT[0], uT[1], (ar_, ai_, x0b), True, ygT[0], ygT[1], xdummy[0], xdummy[1])
    kb.pop()
    phase_glu(g, T, ygT[0], ygT[1], I["c_glu_w"], I["c_glu_b"], zT[0], zT[1])
    kb.push()
    mod, mod_b = kb.sb("mod1b", [128, 6 * D], F32)
    for q in range(6):
        kb.dma("sp", mod[:, q * D:(q + 1) * D], modrow[0][q * D:(q + 1) * D].partition_broadcast(128), reads=[modrow[1]], writes=[mod_b])

    def get_xT(ti, xT, xTb):
        kb.dma("sp", xT[:], zT[0][:, :, ti * 128:(ti + 1) * 128], reads=[zT[1]], writes=[xTb])
    phase_outproj_ln_router(g, T, NE, get_xT, hin, I["c_out_w"], None, mod, mod_b, I["ln_g10"], I["ln_b10"],
                            I["router_w1"], I["router_b1"], h1[0], h1[1], xfT[0], xfT[1], gates[0], gates[1], "w_cout")
    kb.pop()
    phase_moe(g, T, NE, xfT[0], xfT[1], gates[0], gates[1], I["exp_w1_1"], I["exp_b1_1"], I["exp_w2_1"], I["exp_b2_1"], accd[0], accd[1])
    kb.push()
    mod, mod_b = kb.sb("mod1c", [128, 6 * D], F32)
    for q in range(6):
        kb.dma("sp", mod[:, q * D:(q + 1) * D], modrow[0][q * D:(q + 1) * D].partition_broadcast(128), reads=[modrow[1]], writes=[mod_b])
    phase_ln2(g, T, h1[0], h1[1], accd[0], accd[1], mod, mod_b, I["ln_g11"], I["ln_b11"], out_d, out_b)
    kb.pop()


S5_SPECS = {"c_A_re": [128, 64], "c_A_im": [128, 64], "c_log_dt": [128], "c_B_re": [128, 64, 16], "c_B_im": [128, 64, 16],
            "c_C_re": [128, 16, 64], "c_C_im": [128, 16, 64], "c_D": [128, 16]}


def l1_front_specs():
    sp = {"ada_w1": ([D, 6 * D], F32), "ada_b1": ([6 * D], F32), "c_in_w": ([D, D], F32)}
    for n, shp in S5_SPECS.items():
        sp[n] = (shp, F32)
    return sp


def build_launch1(T, NE, debug=False):
    sp = l0_input_specs(T, NE)
    sp.update(l1_front_specs())
    g = mk_ctx(sp, {"hmid": ([T, D], F32), "xend": ([128, 64, 2], F32)})
    g.debug = debug
    load_consts(g)
    hb = g.kb.buf("hmid")
    layer0(g, T, NE, g.din, g.dout["hmid"], hb)
    g.kb.barrier()
    modrow, uT = layer1_front(g, T, g.din, g.dout["hmid"], hb)
    g.kb.push()
    tb = s5_setup(g, g.din, T, False)
    s5_scan(g, T, tb, uT[0], uT[1], None, False, None, None, g.dout["xend"], g.kb.buf("xend"))
    g.kb.pop()
    g.kb.close()
    return g.nc


def build_launch2(T, NE, debug=False):
    sp = {"hmid": ([T, D], F32), "c": ([D], F32), "xe_prev": ([3, 128, 64, 2], F32),
          "c_glu_w": ([D, D], F32), "c_glu_b": ([D], F32), "c_out_w": ([D, D], F32),
          "ln_g10": ([D], F32), "ln_b10": ([D], F32), "ln_g11": ([D], F32), "ln_b11": ([D], F32),
          "router_w1": ([D, NE], F32), "router_b1": ([NE], F32),
          "exp_w1_1": ([NE, D, D], F32), "exp_b1_1": ([NE, D], F32), "exp_w2_1": ([NE, D // 2, D], F32), "exp_b2_1": ([NE, D], F32)}
    sp.update(l1_front_specs())
    for n, shp in CONST_SHAPES.items():
        sp[n] = (shp, F32)
    g = mk_ctx(sp, {"out": ([T, D], F32)})
    g.debug = debug
    load_consts(g)
    hb = g.kb.buf("hmid_in")
    modrow, uT = layer1_front(g, T, g.din, g.din["hmid"], hb)
    layer1_back(g, T, NE, g.din, modrow, uT, g.din["hmid"], hb, g.din["xe_prev"], g.dout["out"], g.kb.buf("out"))
    g.kb.close()
    return g.nc


def l1_common_maps(inp):
    return {"ada_w1": inp["ada_w"][1], "ada_b1": inp["ada_b"][1], "c_in_w": inp["c_in_w"][0],
            "c_A_re": inp["c_A_re"][0], "c_A_im": inp["c_A_im"][0], "c_log_dt": inp["c_log_dt"][0],
            "c_B_re": inp["c_B_re"][0], "c_B_im": inp["c_B_im"][0], "c_C_re": inp["c_C_re"][0], "c_C_im": inp["c_C_im"][0],
            "c_D": inp["c_D"][0]}


def run_all(inp, B, S, NCORES, NE, debug=False):
    cpb = NCORES // B
    maps1, T = shard_l0(inp, B, S, NCORES, NE)
    l1c = {k: np.ascontiguousarray(v) for k, v in l1_common_maps(inp).items()}
    for m in maps1:
        m.update(l1c)
    nc1 = build_launch1(T, NE, debug)
    r1 = run_bass_kernel_spmd(nc1, maps1, core_ids=list(range(NCORES))).results
    consts = host_consts()
    maps2 = []
    for core in range(NCORES):
        b, k = core // cpb, core % cpb
        xe = np.zeros((3, 128, 64, 2), np.float32)
        for d_ in range(3):
            if k - 1 - d_ >= 0:
                xe[d_] = r1[core - 1 - d_]["xend"]
        m = {"hmid": r1[core]["hmid"], "c": inp["c"][b], "xe_prev": xe,
             "c_glu_w": inp["c_glu_w"][0], "c_glu_b": inp["c_glu_b"][0], "c_out_w": inp["c_out_w"][0],
             "ln_g10": inp["ln_g"][1, 0], "ln_b10": inp["ln_b"][1, 0], "ln_g11": inp["ln_g"][1, 1], "ln_b11": inp["ln_b"][1, 1],
             "router_w1": inp["router_w"][1], "router_b1": inp["router_b"][1],
             "exp_w1_1": inp["exp_w1"][1], "exp_b1_1": inp["exp_b1"][1], "exp_w2_1": inp["exp_w2"][1], "exp_b2_1": inp["exp_b2"][1]}
        m.update(l1c)
        m.update(consts)
        maps2.append({k_: np.ascontiguousarray(v) for k_, v in m.items()})
    nc2 = build_launch2(T, NE, debug)
    r2 = run_bass_kernel_spmd(nc2, maps2, core_ids=list(range(NCORES))).results
    out = np.concatenate([r["out"] for r in r2], 0).reshape(B, S, D)
    return out, r1, r2


def kernel(**inputs):
    inp = {k: np.asarray(v) for k, v in inputs.items()}
    B, S = inp["x"].shape[0], inp["x"].shape[1]
    NE = inp["router_w"].shape[2]
    out, _, _ = run_all(inp, B, S, 8, NE)
    return out.astype(np.float32)
```
